# Optimizing a Trainium2 kernel written in Bass

```python
import jax, jax.numpy as jnp
from jax import lax
import numpy as np

D_MODEL = 1024
BATCH = 2
SEQ = 16384
DEPTH = 1
DEC_BATCH = 4
DEC_SEQ = 8192
PAST_LEN = 128

D_CONV = 512
CONV_WIDTH = 3
N_HEADS = 8
QK_NOPE = 64
QK_ROPE = 32
V_HEAD = 64
Q_LORA = 256
KV_LORA = 128
QK_HEAD = QK_NOPE + QK_ROPE
ROPE_THETA = 10000.0
Q_BLOCK = 128
PEER_HEADS = 8
N_KEYS = 128
N_EXPERTS = N_KEYS * N_KEYS
PEER_DK = 128
PEER_TOPK = 16
PEER_CHUNK = 128
EPS = 1e-6
IN_COLS = 3 * D_CONV + Q_LORA + KV_LORA + QK_ROPE + 2 * D_MODEL

kernel_name = "hybrid_conv_mla_peer_encoder"


def rmsnorm(x, g):
    xf = x.astype(jnp.float32)
    y = xf * lax.rsqrt(jnp.mean(xf * xf, axis=-1, keepdims=True) + EPS)
    return (y * g.astype(jnp.float32)).astype(x.dtype)


def rotary_tables(S, dtype):
    pos = jnp.arange(S, dtype=jnp.float32)
    inv = ROPE_THETA ** (-jnp.arange(0, QK_ROPE, 2, dtype=jnp.float32) / QK_ROPE)
    ang = pos[:, None] * inv[None, :]
    return jnp.cos(ang).astype(dtype), jnp.sin(ang).astype(dtype)


def apply_rope(t, cos, sin):
    c = cos[None, :, None, :]
    s = sin[None, :, None, :]
    t1, t2 = jnp.split(t, 2, axis=-1)
    return jnp.concatenate([t1 * c - t2 * s, t1 * s + t2 * c], axis=-1)


def block_attention(q, k, v):
    B, S, H, Dh = q.shape
    nb = S // Q_BLOCK
    qb = q.reshape(B, nb, Q_BLOCK, H, Dh).transpose(1, 0, 2, 3, 4)

    def one_block(qi):
        s = jnp.einsum('bqhd,bkhd->bhqk', qi, k, preferred_element_type=jnp.float32)
        p = jax.nn.softmax(s, axis=-1).astype(v.dtype)
        return jnp.einsum('bhqk,bkhd->bqhd', p, v)

    out = lax.map(one_block, qb)
    return out.transpose(1, 0, 2, 3, 4).reshape(B, S, H * V_HEAD)


def peer(xn, w_pq, k1, k2, u_tab, v_tab):
    B, S, D = xn.shape
    xt = xn.reshape((B * S) // PEER_CHUNK, PEER_CHUNK, D)
    half = PEER_DK // 2

    def one_chunk(xc):
        q = (xc @ w_pq).reshape(PEER_CHUNK, PEER_HEADS, PEER_DK)
        s1 = jnp.einsum('chd,hnd->chn', q[..., :half], k1)
        s2 = jnp.einsum('chd,hnd->chn', q[..., half:], k2)
        v1, i1 = lax.top_k(s1, PEER_TOPK)
        v2, i2 = lax.top_k(s2, PEER_TOPK)
        comb = (v1[..., :, None] + v2[..., None, :]).reshape(PEER_CHUNK, PEER_HEADS, PEER_TOPK * PEER_TOPK)
        sc, pos = lax.top_k(comb, PEER_TOPK)
        ia = pos // PEER_TOPK
        ib = pos % PEER_TOPK
        e = (jnp.take_along_axis(i1, ia, axis=-1) * N_KEYS
             + jnp.take_along_axis(i2, ib, axis=-1))
        g = jax.nn.softmax(sc.astype(jnp.float32), axis=-1).astype(xc.dtype)
        a = jax.nn.gelu(jnp.einsum('chkd,cd->chk', u_tab[e], xc))
        return jnp.einsum('chk,chkd->cd', g * a, v_tab[e])

    return lax.map(one_chunk, xt).reshape(B, S, D)


def encoder_layer(x, c, w_ada, b_ada, g_norm1, w_in, conv_w, w_conv_out, g_q_lora, w_uq,
                  g_kv_lora, w_ukv, g_qnorm, g_knorm, w_attn_out, w_out, g_norm2,
                  peer_wq, peer_k1, peer_k2, peer_u, peer_v):
    B, S, D = x.shape
    ada = (c @ w_ada + b_ada)[:, None, :]
    shift1, scale1, gate1, shift2, scale2, gate2 = jnp.split(ada, 6, axis=-1)

    h = rmsnorm(x, g_norm1) * (1.0 + scale1) + shift1
    proj = h @ w_in
    o1 = 3 * D_CONV
    o2 = o1 + Q_LORA
    o3 = o2 + KV_LORA
    o4 = o3 + QK_ROPE
    o5 = o4 + D_MODEL
    b_g, c_g, hc, cq, ckv, kr, ga, gb = jnp.split(
        proj, [D_CONV, 2 * D_CONV, o1, o2, o3, o4, o5], axis=-1)

    z = c_g * hc
    zp = jnp.pad(z, ((0, 0), (1, 1), (0, 0)))
    y = zp[:, :-2] * conv_w[0] + zp[:, 1:-1] * conv_w[1] + zp[:, 2:] * conv_w[2]
    out_a = (b_g * y) @ w_conv_out

    q = (rmsnorm(cq, g_q_lora) @ w_uq).reshape(B, S, N_HEADS, QK_HEAD)
    kv = (rmsnorm(ckv, g_kv_lora) @ w_ukv).reshape(B, S, N_HEADS, QK_NOPE + V_HEAD)
    k_nope, v = kv[..., :QK_NOPE], kv[..., QK_NOPE:]
    k_r = jnp.broadcast_to(kr[:, :, None, :], (B, S, N_HEADS, QK_ROPE))
    k = jnp.concatenate([k_nope, k_r], axis=-1)
    q = rmsnorm(q, g_qnorm)
    k = rmsnorm(k, g_knorm)
    cos, sin = rotary_tables(S, x.dtype)
    q = jnp.concatenate([q[..., :QK_NOPE], apply_rope(q[..., QK_NOPE:], cos, sin)], axis=-1)
    k = jnp.concatenate([k[..., :QK_NOPE], apply_rope(k[..., QK_NOPE:], cos, sin)], axis=-1)
    out_b = block_attention(q * (QK_HEAD ** -0.5), k, v) @ w_attn_out

    merged = jax.nn.sigmoid(ga) * out_a + jax.nn.sigmoid(gb) * out_b
    x = x + gate1 * (merged @ w_out)

    h2 = rmsnorm(x, g_norm2) * (1.0 + scale2) + shift2
    x = x + gate2 * peer(h2, peer_wq, peer_k1, peer_k2, peer_u, peer_v)
    return x


def setup_inputs(seed: int = 0) -> dict:
    key = jax.random.key(seed)
    ks = jax.random.split(key, 32)
    f32 = jnp.float32

    def nrm(k, shape, scale):
        return jax.random.normal(k, shape, dtype=f32) * scale

    def gain(k, n):
        return 1.0 + 0.05 * jax.random.normal(k, (DEPTH, n), dtype=f32)

    L = DEPTH
    return {
        "x_prompt": nrm(ks[0], (BATCH, SEQ, D_MODEL), 1.0),
        "x_sample": nrm(ks[1], (DEC_BATCH, DEC_SEQ, D_MODEL), 1.0),
        "c_prompt": nrm(ks[2], (BATCH, D_MODEL), 1.0),
        "c_sample": nrm(ks[3], (DEC_BATCH, D_MODEL), 1.0),
        "w_ada": nrm(ks[4], (L, D_MODEL, 6 * D_MODEL), 0.3 * D_MODEL ** -0.5),
        "b_ada": nrm(ks[5], (L, 6 * D_MODEL), 0.02),
        "g_norm1": gain(ks[6], D_MODEL),
        "w_in": nrm(ks[7], (L, D_MODEL, IN_COLS), D_MODEL ** -0.5),
        "conv_w": nrm(ks[8], (L, CONV_WIDTH, D_CONV), CONV_WIDTH ** -0.5),
        "w_conv_out": nrm(ks[9], (L, D_CONV, D_MODEL), D_CONV ** -0.5),
        "g_q_lora": gain(ks[10], Q_LORA),
        "w_uq": nrm(ks[11], (L, Q_LORA, N_HEADS * QK_HEAD), Q_LORA ** -0.5),
        "g_kv_lora": gain(ks[12], KV_LORA),
        "w_ukv": nrm(ks[13], (L, KV_LORA, N_HEADS * (QK_NOPE + V_HEAD)), KV_LORA ** -0.5),
        "g_qnorm": gain(ks[14], QK_HEAD),
        "g_knorm": gain(ks[15], QK_HEAD),
        "w_attn_out": nrm(ks[16], (L, N_HEADS * V_HEAD, D_MODEL), (N_HEADS * V_HEAD) ** -0.5),
        "w_out": nrm(ks[17], (L, D_MODEL, D_MODEL), D_MODEL ** -0.5),
        "g_norm2": gain(ks[18], D_MODEL),
        "peer_wq": nrm(ks[19], (L, D_MODEL, PEER_HEADS * PEER_DK), D_MODEL ** -0.5),
        "peer_k1": nrm(ks[20], (L, PEER_HEADS, N_KEYS, PEER_DK // 2), (PEER_DK // 2) ** -0.5),
        "peer_k2": nrm(ks[21], (L, PEER_HEADS, N_KEYS, PEER_DK // 2), (PEER_DK // 2) ** -0.5),
        "peer_u": nrm(ks[22], (L, N_EXPERTS, D_MODEL), D_MODEL ** -0.5),
        "peer_v": nrm(ks[23], (L, N_EXPERTS, D_MODEL), PEER_HEADS ** -0.5),
    }


def reference(x_prompt, x_sample, c_prompt, c_sample, w_ada, b_ada, g_norm1, w_in, conv_w,
              w_conv_out, g_q_lora, w_uq, g_kv_lora, w_ukv, g_qnorm, g_knorm, w_attn_out,
              w_out, g_norm2, peer_wq, peer_k1, peer_k2, peer_u, peer_v):
    layer_params = (w_ada, b_ada, g_norm1, w_in, conv_w, w_conv_out, g_q_lora, w_uq,
                    g_kv_lora, w_ukv, g_qnorm, g_knorm, w_attn_out, w_out, g_norm2,
                    peer_wq, peer_k1, peer_k2, peer_u, peer_v)
    y_prompt = x_prompt
    y_sample = x_sample
    for l in range(DEPTH):
        p_l = [p[l] for p in layer_params]
        y_prompt = encoder_layer(y_prompt, c_prompt, *p_l)
        y_sample = encoder_layer(y_sample, c_sample, *p_l)
    return (y_prompt, y_sample)
```

```python
import contextlib
import numpy as np
import concourse.bass as bass
import concourse.mybir as mybir
from concourse.bass_utils import run_bass_kernel_spmd

F32 = mybir.dt.float32
BF16 = mybir.dt.bfloat16
U32 = mybir.dt.uint32
AF = mybir.ActivationFunctionType
ALU = mybir.AluOpType
AX = mybir.AxisListType

D = 1024
DC = 8
EPS = 1e-6
NE = 16384
NCH = 128
O_CQ = 1536
O_CKV = 1792
O_GA = 1952
O_GB = 2976
NEG = -1.0e30
TINY_TABLES = False


class _Stop(Exception):
    pass


STOPPED = [False]


def chk(n):
    import os
    if int(os.environ.get("K_STOP", "100000")) == n:
        if n >= 100:
            STOPPED[0] = True
        else:
            raise _Stop()


class Buf:
    __slots__ = ("name", "last_w", "readers", "excl")

    def __init__(self, name, excl=False):
        self.name = name
        self.last_w = None
        self.readers = []
        self.excl = excl


class T:
    __slots__ = ("t", "b", "name")

    def __init__(self, t, b, name):
        self.t = t
        self.b = b
        self.name = name


class Ring:
    def __init__(self, tiles):
        self.tiles = tiles
        self.i = 0

    def get(self):
        t = self.tiles[self.i % len(self.tiles)]
        self.i += 1
        return t


class FW:
    def __init__(self, nc):
        self.nc = nc
        self.names = ["sp", "pe", "dve", "act", "pool"]
        self.sem = {k: nc.alloc_semaphore(name="s_" + k) for k in self.names}
        self.cnt = {k: 0 for k in self.names}
        self.waited = {k: {} for k in self.names}
        self.dsems = {}
        self.dcnt = {}
        self.semobj = dict(self.sem)
        self.prog = {k: [] for k in self.names}
        self.bufs = []
        self.n_inst = 0

    def buf(self, name, excl=False):
        b = Buf(name, excl)
        self.bufs.append(b)
        return b

    def _needs(self, reads, writes):
        needs = {}

        def add(ev):
            if ev is None:
                return
            k = ev[0]
            if k not in needs or needs[k] < ev[1]:
                needs[k] = ev[1]
        for b in reads:
            add(b.last_w)
        for b in writes:
            add(b.last_w)
            for r in b.readers:
                add(r)
        return needs

    def _do_waits(self, eng, needs):
        w = self.waited[eng]
        for k, val in needs.items():
            if k == eng:
                if eng in ("pe", "sp"):
                    continue
            if w.get(k, 0) >= val:
                continue
            self.prog[eng].append(("w", self.semobj[k], val))
            w[k] = val

    def _record(self, ev, reads, writes):
        for b in reads:
            rs = b.readers
            for i, r in enumerate(rs):
                if r[0] == ev[0]:
                    rs[i] = ev
                    break
            else:
                rs.append(ev)
        for b in writes:
            b.last_w = ev
            b.readers = []

    def op(self, eng, fn, R=(), W=()):
        if STOPPED[0]:
            return
        if any(b.excl for b in R):
            W = list(W) + [b for b in R if b.excl]
            R = [b for b in R if not b.excl]
        needs = self._needs(R, W)
        self._do_waits(eng, needs)
        self.cnt[eng] += 1
        self.prog[eng].append(("i", fn, self.sem[eng], 1))
        ev = (eng, self.cnt[eng])
        self._record(ev, R, W)
        self.n_inst += 1

    def dma(self, q, out, in_, R=(), W=(), key=None):
        if STOPPED[0]:
            return
        if key not in self.dsems:
            s = self.nc.alloc_semaphore(name="d_" + str(key))
            self.dsems[key] = s
            self.semobj[("d", key)] = s
            self.dcnt[key] = 0
        needs = self._needs(R, W)
        self._do_waits(q, needs)
        self.prog[q].append(("i", (lambda e, out=out, in_=in_: e.dma_start(out=out, in_=in_)),
                             self.dsems[key], 16))
        self.dcnt[key] += 16
        ev = (("d", key), self.dcnt[key])
        self._record(ev, R, W)
        self.n_inst += 1

    def barrier(self):
        allk = [(k, self.cnt[k]) for k in self.names] + [(("d", k), v) for k, v in self.dcnt.items()]
        for e in self.names:
            for k, v in allk:
                if k == e or v == 0:
                    continue
                if self.waited[e].get(k, 0) >= v:
                    continue
                self.prog[e].append(("w", self.semobj[k], v))
                self.waited[e][k] = v
        for b in self.bufs:
            b.last_w = None
            b.readers = []

    def finalize(self):
        nc = self.nc
        prog = self.prog

        def run(e, lst):
            for t in lst:
                if t[0] == "w":
                    e.wait_ge(t[1], t[2])
                else:
                    t[1](e).then_inc(t[2], t[3])
        with nc.Block() as block:
            @block.sync
            def _(e):
                run(e, prog["sp"])

            @block.tensor
            def _(e):
                run(e, prog["pe"])

            @block.vector
            def _(e):
                run(e, prog["dve"])

            @block.scalar
            def _(e):
                run(e, prog["act"])

            @block.gpsimd
            def _(e):
                run(e, prog["pool"])

    def mm(self, out, lhsT, rhs, start, stop, R, W):
        self.op("pe", lambda e: e.matmul(out, lhsT=lhsT, rhs=rhs, start=start, stop=stop), R, W)

    def tr(self, out, in_, ident, R, W):
        self.op("pe", lambda e: e.transpose(out=out, in_=in_, identity=ident), R, W)

    def act(self, out, in_, func, R, W, bias=None, scale=None, accum_out=None):
        kw = {}
        if bias is not None:
            kw["bias"] = bias
        if scale is not None:
            kw["scale"] = scale
        if accum_out is not None:
            kw["accum_out"] = accum_out
        self.op("act", lambda e: e.activation(out=out, in_=in_, func=func, **kw), R, W)

    def tt(self, eng, out, in0, in1, op, R, W):
        self.op(eng, lambda e: e.tensor_tensor(out=out, in0=in0, in1=in1, op=op), R, W)

    def ts(self, eng, out, in0, s1, s2, op0, op1, R, W):
        if op1 is None:
            self.op(eng, lambda e: e.tensor_scalar(out=out, in0=in0, scalar1=s1, scalar2=None, op0=op0), R, W)
        else:
            self.op(eng, lambda e: e.tensor_scalar(out=out, in0=in0, scalar1=s1, scalar2=s2, op0=op0, op1=op1), R, W)

    def stt(self, out, in0, scalar, in1, op0, op1, R, W):
        self.op("dve", lambda e: e.scalar_tensor_tensor(out=out, in0=in0, scalar=scalar, in1=in1, op0=op0, op1=op1), R, W)

    def cp(self, eng, out, in_, R, W):
        if eng == "act":
            self.op("act", lambda e: e.activation(out=out, in_=in_, func=AF.Copy), R, W)
        else:
            self.op(eng, lambda e: e.tensor_copy(out=out, in_=in_), R, W)

    def red(self, out, in_, op, R, W):
        self.op("dve", lambda e: e.tensor_reduce(out=out, in_=in_, axis=AX.X, op=op), R, W)

    def recip(self, out, in_, R, W):
        self.op("dve", lambda e: e.reciprocal(out=out, in_=in_), R, W)

    def ms(self, eng, ap, val, W):
        self.op(eng, lambda e: e.memset(ap, val), (), W)


def bcl(ap, n):
    sh = list(ap.shape)
    return ap.unsqueeze(len(sh)).to_broadcast(sh + [n])


def build(NQ, NKP, NKS, phases=None):
    import os
    if phases is None:
        phases = os.environ.get('K_PHASES', 'PH0,P0,PH1,PH2,PH3,PH4,PH5').split(',')
    nc = bass.Bass("TRN2", target_bir_lowering=False)
    fw = FW(nc)
    NKs = [NKP, NKS]

    def din(name, shape, dt=F32):
        return nc.dram_tensor(name, list(shape), dt, kind="ExternalInput").ap()

    def dscr(name, shape, dt):
        return nc.dram_tensor(name, list(shape), dt, kind="Internal").ap()

    xk = [din("xk_p", [NKP, D]), din("xk_s", [NKS, D])]
    xq = [din("xq_p", [NQ + 2, D]), din("xq_s", [NQ + 2, D])]
    hm_d = din("hmask", [128, 2, 2])
    cT_d = din("cT2", [128, DC, 2])
    cosk = [din("cosk_p", [NKP, 16]), din("cosk_s", [NKS, 16])]
    sink = [din("sink_p", [NKP, 16]), din("sink_s", [NKS, 16])]
    cosq = [din("cosq_p", [NQ, 16]), din("cosq_s", [NQ, 16])]
    sinq = [din("sinq_p", [NQ, 16]), din("sinq_s", [NQ, 16])]
    w_ada_d = din("w_ada", [D, 6 * D])
    b_ada_d = din("b_ada2", [2, 6 * D])
    g1T_d = din("g1T", [128, DC])
    g2T_d = din("g2T", [128, DC])
    w_in_d = din("w_in", [D, 4000])
    convw_d = din("convw", [128, 4, 3])
    wco_d = din("w_conv_out", [512, D])
    gq_d = din("gq_bc", [128, 256])
    wuq_d = din("w_uq", [256, 768])
    gkv_d = din("gkv_bc", [128, 128])
    wukv_d = din("w_ukv", [128, D])
    gqn_d = din("gqn_bc", [128, 96])
    gkn_d = din("gkn_bc", [128, 96])
    wao_d = din("w_attn_out", [512, D])
    wout_d = din("w_out", [D, D])
    wq_d = din("peer_wq", [D, D])
    k12_d = din("k12T", [128, 8, 128])
    if TINY_TABLES:
        ut_d = din("UT", [1, 128, D])
        v_d = din("peer_v", [128, D])
    else:
        ut_d = din("UT", [NCH, 128, D])
        v_d = din("peer_v", [NE, D])
    ident_d = din("ident", [128, 128])
    iota_d = din("iota", [128, 128])
    selA_d = din("selA", [2, 2, 128])
    sel65_d = din("sel65", [65, 64])
    out_d = [nc.dram_tensor("out_p", [NQ, D], F32, kind="ExternalOutput").ap(),
             nc.dram_tensor("out_s", [NQ, D], F32, kind="ExternalOutput").ap()]

    KT = [dscr("KT_p", [8, 97, NKP], BF16), dscr("KT_s", [8, 97, NKS], BF16)]
    VP = [dscr("VP_p", [8, 128, NKP // 128, 66], BF16), dscr("VP_s", [8, 128, NKS // 128, 66], BF16)]
    QT = [dscr("QT_p", [8, 97, NQ], BF16), dscr("QT_s", [8, 97, NQ], BF16)]
    MA = [dscr("MA_p", [8, 128, NQ], BF16), dscr("MA_s", [8, 128, NQ], BF16)]
    SGB = [dscr("SGB_p", [8, 128, NQ], BF16), dscr("SGB_s", [8, 128, NQ], BF16)]
    OB = [dscr("OB_p", [8, 64, NQ], BF16), dscr("OB_s", [8, 64, NQ], BF16)]
    Y1 = [dscr("Y1_p", [NQ, D], F32), dscr("Y1_s", [NQ, D], F32)]
    GATE = dscr("GATE", [2, 2, 128, D], F32)
    UTb = dscr("UTb", [NCH, 128, D], BF16)
    Vb = dscr("Vb", [NE, D], BF16)

    uid = [0]

    with contextlib.ExitStack() as top:
        def sb(es, name, shape, dt):
            uid[0] += 1
            nm = "%s_%d" % (name, uid[0])
            t = es.enter_context(nc.sbuf_tensor(nm, list(shape), dt))
            return T(t, fw.buf(nm), nm)

        def ring(es, name, shape, dt, n):
            return Ring([sb(es, "%s%d" % (name, i), shape, dt) for i in range(n)])

        psum = top.enter_context(nc.psum_tensor("psum", [128, 8, 512], F32))
        pbank = [fw.buf("bank%d" % i, True) for i in range(8)]

        def bank_f(i):
            return psum[:, i, :]

        def bank_b(i):
            return psum[:, i, :].bitcast(BF16)

        ident_f = sb(top, "identf", [128, 128], F32)
        ident_b = sb(top, "identb", [128, 128], BF16)
        iota_f = sb(top, "iota", [128, 128], F32)
        selA = sb(top, "selA", [2, 2, 128], F32)
        sel65 = sb(top, "sel65", [65, 64], F32)
        hm = sb(top, "hm", [128, 2, 2], F32)
        gq_bc = sb(top, "gq", [128, 256], F32)
        gkv_bc = sb(top, "gkv", [128, 128], F32)
        gqs_bc = sb(top, "gqs", [128, 96], F32)
        gkn_bc = sb(top, "gkn", [128, 96], F32)
        convw = sb(top, "convw", [128, 4, 3], F32)
        negB = sb(top, "negB", [128, 1], F32)
        gmod1 = [sb(top, "gmod1_%d" % r, [128, DC], F32) for r in range(2)]
        shift1 = [sb(top, "shift1_%d" % r, [128, DC], F32) for r in range(2)]
        gmod2 = [sb(top, "gmod2_%d" % r, [128, DC], F32) for r in range(2)]
        shift2 = [sb(top, "shift2_%d" % r, [128, DC], F32) for r in range(2)]
        wkv_b = sb(top, "wkv_b", [128, DC, 160], BF16)
        wukv_b = sb(top, "wukv_b", [128, D], BF16)

        def load(dst, src_ap, q="sp"):
            fw.dma(q, dst.t[:], src_ap, W=[dst.b], key=dst.name)

        with contextlib.ExitStack() as ph:
            load(ident_f, ident_d)
            load(iota_f, iota_d)
            load(selA, selA_d)
            load(sel65, sel65_d)
            load(hm, hm_d)
            load(gq_bc, gq_d)
            load(gkv_bc, gkv_d)
            load(gkn_bc, gkn_d)
            load(convw, convw_d)
            gqn_bc = sb(ph, "gqn", [128, 96], F32)
            load(gqn_bc, gqn_d)
            g1T = sb(ph, "g1T", [128, DC], F32)
            g2T = sb(ph, "g2T", [128, DC], F32)
            load(g1T, g1T_d)
            load(g2T, g2T_d)
            cT = sb(ph, "cT", [128, DC, 2], F32)
            load(cT, cT_d)
            bada = sb(ph, "bada", [2, 6 * D], F32)
            load(bada, b_ada_d)
            fw.cp("dve", ident_b.t[:], ident_f.t[:], [ident_f.b], [ident_b.b])
            fw.ts("dve", gqs_bc.t[:], gqn_bc.t[:], 96.0 ** -0.5, None, ALU.mult, None, [gqn_bc.b], [gqs_bc.b])
            sq_t = sb(ph, "sqt", [128, 96], F32)
            m1 = sb(ph, "m1", [128, 1], F32)
            m2 = sb(ph, "m2", [128, 1], F32)
            fw.tt("dve", sq_t.t[:], gqs_bc.t[:], gqs_bc.t[:], ALU.mult, [gqs_bc.b], [sq_t.b])
            fw.red(m1.t[:], sq_t.t[:], ALU.max, [sq_t.b], [m1.b])
            fw.tt("dve", sq_t.t[:], gkn_bc.t[:], gkn_bc.t[:], ALU.mult, [gkn_bc.b, m1.b], [sq_t.b])
            fw.red(m2.t[:], sq_t.t[:], ALU.max, [sq_t.b], [m2.b])
            fw.tt("dve", m1.t[:], m1.t[:], m2.t[:], ALU.mult, [m1.b, m2.b], [m1.b])
            fw.ts("dve", m1.t[:], m1.t[:], 96.0 * 96.0, None, ALU.mult, None, [m1.b], [m1.b])
            fw.act(m1.t[:], m1.t[:], AF.Sqrt, [m1.b], [m1.b])
            fw.ts("dve", negB.t[:], m1.t[:], -1.0, None, ALU.mult, None, [m1.b], [negB.b])

            stg_kv = sb(ph, "stgkv", [128, DC, 160], F32)
            fw.dma("sp", stg_kv.t[:], w_in_d[:, O_CKV:O_CKV + 160].rearrange("(c p) n -> p c n", p=128),
                   W=[stg_kv.b], key=stg_kv.name)
            fw.cp("dve", wkv_b.t[:], stg_kv.t[:], [stg_kv.b], [wkv_b.b])
            stg_ukv = sb(ph, "stgukv", [128, D], F32)
            load(stg_ukv, wukv_d)
            fw.cp("dve", wukv_b.t[:], stg_ukv.t[:], [stg_ukv.b], [wukv_b.b])

            ada = sb(ph, "ada", [2, 6 * D], F32)
            stg = ring(ph, "stgada", [128, DC, 512], F32, 2)
            for g in range(12):
                st = stg.get()
                fw.dma("sp", st.t[:], w_ada_d[:, g * 512:(g + 1) * 512].rearrange("(c p) n -> p c n", p=128),
                       W=[st.b], key=st.name)
                bk = g % 2
                for c in range(DC):
                    fw.mm(psum[0:2, bk, :], cT.t[:, c, :], st.t[:, c, :], c == 0, c == DC - 1,
                          [cT.b, st.b], [pbank[bk]])
                fw.tt("dve", ada.t[:, g * 512:(g + 1) * 512], psum[0:2, bk, :], bada.t[:, g * 512:(g + 1) * 512],
                      ALU.add, [pbank[bk], bada.b], [ada.b])
            adaT = sb(ph, "adaT", [128, 48, 2], F32)
            gst = ring(ph, "gst", [128, D], F32, 2)
            pv = psum[:, 2, 0:96].rearrange("p (n r) -> p n r", r=2)
            for n in range(48):
                fw.tr(pv[:, n, :], ada.t[0:2, n * 128:(n + 1) * 128], ident_f.t[0:2, 0:2],
                      [ada.b, ident_f.b], [pbank[2]])
            fw.cp("dve", adaT.t[:], pv, [pbank[2]], [adaT.b])
            for r in range(2):
                fw.stt(gmod1[r].t[:], adaT.t[:, 8:16, r], 1.0, g1T.t[:], ALU.add, ALU.mult,
                       [adaT.b, g1T.b], [gmod1[r].b])
                fw.cp("dve", shift1[r].t[:], adaT.t[:, 0:8, r], [adaT.b], [shift1[r].b])
                fw.stt(gmod2[r].t[:], adaT.t[:, 32:40, r], 1.0, g2T.t[:], ALU.add, ALU.mult,
                       [adaT.b, g2T.b], [gmod2[r].b])
                fw.cp("dve", shift2[r].t[:], adaT.t[:, 24:32, r], [adaT.b], [shift2[r].b])
                for (wh, off) in ((0, 2 * D), (1, 5 * D)):
                    dst = gst.get()
                    for half in range(2):
                        bk = 4 + half
                        fw.mm(psum[:, bk, :], selA.t[:, r, :], ada.t[:, off + half * 512: off + (half + 1) * 512],
                              True, True, [selA.b, ada.b], [pbank[bk]])
                        fw.cp("act", dst.t[:, half * 512:(half + 1) * 512], psum[:, bk, :], [pbank[bk]], [dst.b])
                    fw.dma("act", GATE[r, wh], dst.t[:], R=[dst.b], key=dst.name)
            fw.barrier()
            ph.close()

        def make_fe(es, n=2):
            S = {}
            S["junk"] = sb(es, "fejunk", [128, D], F32)
            S["ssq"] = ring(es, "fessq", [128, 1], F32, max(2, n))
            S["rstd"] = ring(es, "ferstd", [128, 1], F32, max(2, n))
            S["xs"] = ring(es, "fexs", [128, D], BF16, n)
            S["tmp"] = ring(es, "fetmp", [128, DC, 128], F32, n)
            return S

        def fe_gen(S, xt, rows, gm, sh, hT_ap, hT_buf, bk):
            ssq = S["ssq"].get()
            rstd = S["rstd"].get()
            xs = S["xs"].get()
            tmp = S["tmp"].get()
            junk = S["junk"]
            fw.act(junk.t[:rows], xt.t[:rows], AF.Square, [xt.b], [junk.b, ssq.b], accum_out=ssq.t[:rows])
            yield
            fw.ts("dve", rstd.t[:rows], ssq.t[:rows], 1.0 / D, EPS, ALU.mult, ALU.add, [ssq.b], [rstd.b])
            yield
            fw.act(rstd.t[:rows], rstd.t[:rows], AF.Sqrt, [rstd.b], [rstd.b])
            yield
            fw.recip(rstd.t[:rows], rstd.t[:rows], [rstd.b], [rstd.b])
            yield
            fw.act(xs.t[:rows], xt.t[:rows], AF.Copy, [xt.b, rstd.b], [xs.b], scale=rstd.t[:rows])
            yield
            pv = bank_b(bk).rearrange("p (c t) -> p c t", t=128)
            for c in range(DC):
                fw.tr(pv[:, c, :rows], xs.t[:rows, c * 128:(c + 1) * 128], ident_b.t[:rows, :rows],
                      [xs.b, ident_b.b], [pbank[bk]])
            yield
            fw.tt("dve", tmp.t[:, :, :rows], pv[:, :, :rows], bcl(gm.t[:], rows), ALU.mult,
                  [pbank[bk], gm.b], [tmp.b])
            yield
            fw.tt("pool", hT_ap, tmp.t[:, :, :rows], bcl(sh.t[:], rows), ALU.add, [tmp.b, sh.b], [hT_buf])
            yield

        def fe(S, xt, rows, gm, sh, hT_ap, hT_buf, bk):
            for _ in fe_gen(S, xt, rows, gm, sh, hT_ap, hT_buf, bk):
                pass

        def rms_small_gen(S, src_ap, src_bufs, n, rows=128):
            ssq = S["ssq"].get()
            rstd = S["rstd"].get()
            junk = S["junk"]
            fw.act(junk.t[:rows, 0:n], src_ap, AF.Square, src_bufs, [junk.b, ssq.b], accum_out=ssq.t[:rows])
            yield
            fw.ts("dve", rstd.t[:rows], ssq.t[:rows], 1.0 / n, EPS, ALU.mult, ALU.add, [ssq.b], [rstd.b])
            yield
            fw.act(rstd.t[:rows], rstd.t[:rows], AF.Sqrt, [rstd.b], [rstd.b])
            yield
            fw.recip(rstd.t[:rows], rstd.t[:rows], [rstd.b], [rstd.b])
            yield
            return rstd

        def rms_small(S, src_ap, src_bufs, n, rows=128):
            g = rms_small_gen(S, src_ap, src_bufs, n, rows)
            while True:
                try:
                    next(g)
                except StopIteration as e:
                    return e.value

        def make_qkn(es, n=2):
            S = {}
            S["junk"] = ring(es, "qkjunk", [128, 8, 96], F32, n)
            S["ssq"] = ring(es, "qkssq", [128, 8], F32, n)
            S["xn"] = ring(es, "qkxn", [128, 8, 96], F32, n)
            S["ra"] = ring(es, "qkra", [128, 8, 16], F32, 2 * n)
            S["rb"] = ring(es, "qkrb", [128, 8, 16], F32, 2 * n)
            S["cs"] = ring(es, "qkcs", [128, 2, 16], F32, n + 1)
            return S

        def qknorm_rope_gen(S, xf, g_bc, cos_d, sin_d, out_ap, out_buf):
            cs = S["cs"].get()
            fw.dma("sp", cs.t[:, 0, :], cos_d, W=[cs.b], key=cs.name)
            yield
            fw.dma("sp", cs.t[:, 1, :], sin_d, W=[cs.b], key=cs.name)
            yield
            junk = S["junk"].get()
            ssq = S["ssq"].get()
            xn = S["xn"].get()
            fw.act(junk.t[:], xf.t[:], AF.Square, [xf.b], [junk.b])
            yield
            fw.red(ssq.t[:], junk.t[:], ALU.add, [junk.b], [ssq.b])
            yield
            fw.ts("dve", ssq.t[:], ssq.t[:], 1.0 / 96, EPS, ALU.mult, ALU.add, [ssq.b], [ssq.b])
            yield
            fw.act(ssq.t[:], ssq.t[:], AF.Sqrt, [ssq.b], [ssq.b])
            yield
            fw.recip(ssq.t[:], ssq.t[:], [ssq.b], [ssq.b])
            yield
            fw.tt("dve", xn.t[:], xf.t[:], bcl(ssq.t[:], 96), ALU.mult, [xf.b, ssq.b], [xn.b])
            yield
            fw.tt("dve", xn.t[:], xn.t[:], g_bc.t[:, None, :].to_broadcast([128, 8, 96]), ALU.mult,
                  [xn.b, g_bc.b], [xn.b])
            yield
            fw.cp("act", out_ap[:, :, 0:64], xn.t[:, :, 0:64], [xn.b], [out_buf])
            yield
            ra = S["ra"].get()
            rb = S["rb"].get()
            cb = cs.t[:, 0:1, :].to_broadcast([128, 8, 16])
            sbb = cs.t[:, 1:2, :].to_broadcast([128, 8, 16])
            t1 = xn.t[:, :, 64:80]
            t2 = xn.t[:, :, 80:96]
            fw.tt("pool", ra.t[:], t1, cb, ALU.mult, [xn.b, cs.b], [ra.b])
            yield
            fw.tt("pool", rb.t[:], t2, sbb, ALU.mult, [xn.b, cs.b], [rb.b])
            yield
            fw.tt("pool", out_ap[:, :, 64:80], ra.t[:], rb.t[:], ALU.subtract, [ra.b, rb.b], [out_buf])
            yield
            ra = S["ra"].get()
            rb = S["rb"].get()
            fw.tt("pool", ra.t[:], t1, sbb, ALU.mult, [xn.b, cs.b], [ra.b])
            yield
            fw.tt("pool", rb.t[:], t2, cb, ALU.mult, [xn.b, cs.b], [rb.b])
            yield
            fw.tt("pool", out_ap[:, :, 80:96], ra.t[:], rb.t[:], ALU.add, [ra.b, rb.b], [out_buf])
            yield

        def qknorm_rope(S, xf, g_bc, cos_d, sin_d, out_ap, out_buf):
            for _ in qknorm_rope_gen(S, xf, g_bc, cos_d, sin_d, out_ap, out_buf):
                pass

        for ph in ([contextlib.ExitStack()] if 'P0' in phases else []):
            su = ring(ph, "p0su", [128, D], F32, 3)
            sv = ring(ph, "p0sv", [128, D], F32, 3)
            bu = ring(ph, "p0bu", [128, D], BF16, 3)
            bv = ring(ph, "p0bv", [128, D], BF16, 3)
            for c in range(NCH):
                a = su.get()
                b = bu.get()
                fw.dma("sp", a.t[:], ut_d[c], W=[a.b], key=a.name)
                fw.cp("act", b.t[:], a.t[:], [a.b], [b.b])
                fw.dma("act", UTb[c], b.t[:], R=[b.b], key=b.name)
                a = sv.get()
                b = bv.get()
                fw.dma("sp", a.t[:], v_d[c * 128:(c + 1) * 128, :], W=[a.b], key=a.name)
                fw.cp("dve", b.t[:], a.t[:], [a.b], [b.b])
                fw.dma("act", Vb[c * 128:(c + 1) * 128, :], b.t[:], R=[b.b], key=b.name)
            fw.barrier()
            ph.close()

        for ph in ([contextlib.ExitStack()] if 'PH1' in phases else []):
            FE = make_fe(ph, 4)
            QK = make_qkn(ph, 2)
            xr = ring(ph, "k_x", [128, D], F32, 3)
            hTr = ring(ph, "k_hT", [128, DC, 128], BF16, 2)
            cn_r = ring(ph, "k_cn", [128, 128], BF16, 2)
            cTr = ring(ph, "k_cT", [128, 128], BF16, 2)
            kf_r = ring(ph, "k_kf", [128, 8, 96], F32, 2)
            kb_r = ring(ph, "k_kb", [128, 8, 96], BF16, 2)
            ktb_r = ring(ph, "k_ktb", [97, 8, 512], BF16, 2)
            vpb_r = ring(ph, "k_vpb", [128, 8, 4, 66], BF16, 2)
            for t_ in vpb_r.tiles:
                fw.ms("pool", t_.t[:], 1.0, [t_.b])
            for t_ in ktb_r.tiles:
                fw.ms("pool", t_.t[:], 1.0, [t_.b])

            def k_tile(r, kb, i, ktb, vpb, par):
                X = 0 if par == 0 else 3
                K0 = 1 if par == 0 else 4
                t0 = (kb * 4 + i) * 128
                xt = xr.get()
                fw.dma("sp", xt.t[:], xk[r][t0:t0 + 128, :], W=[xt.b], key=xt.name)
                hT = hTr.get()
                yield from fe_gen(FE, xt, 128, gmod1[r], shift1[r], hT.t[:], hT.b, X)
                pk = psum[:, X, 0:160]
                for c in range(DC):
                    fw.mm(pk, hT.t[:, c, :], wkv_b.t[:, c, :], c == 0, c == DC - 1, [hT.b, wkv_b.b], [pbank[X]])
                yield
                rstd = yield from rms_small_gen(FE, psum[:, X, 0:128], [pbank[X]], 128)
                cn = cn_r.get()
                fw.stt(cn.t[:], psum[:, X, 0:128], rstd.t[:], gkv_bc.t[:], ALU.mult, ALU.mult,
                       [pbank[X], rstd.b, gkv_bc.b], [cn.b])
                kf = kf_r.get()
                fw.cp("dve", kf.t[:, :, 64:96], psum[:, X, 128:160][:, None, :].to_broadcast([128, 8, 32]),
                      [pbank[X]], [kf.b])
                yield
                pT = bank_b(X)[:, 0:128]
                fw.tr(pT, cn.t[:], ident_b.t[:], [cn.b, ident_b.b], [pbank[X]])
                yield
                cTt = cTr.get()
                fw.cp("dve", cTt.t[:], pT, [pbank[X]], [cTt.b])
                yield
                for half in range(2):
                    fw.mm(psum[:, K0 + half, :], cTt.t[:], wukv_b.t[:, half * 512:(half + 1) * 512], True, True,
                          [cTt.b, wukv_b.b], [pbank[K0 + half]])
                yield
                for half in range(2):
                    kvv = psum[:, K0 + half, :].rearrange("p (h d) -> p h d", d=128)
                    hs = slice(half * 4, half * 4 + 4)
                    fw.cp("act", kf.t[:, hs, 0:64], kvv[:, :, 0:64], [pbank[K0 + half]], [kf.b])
                    fw.cp("dve", vpb.t[:, hs, i, 0:64], kvv[:, :, 64:128], [pbank[K0 + half]], [vpb.b])
                yield
                kbt = kb_r.get()
                yield from qknorm_rope_gen(QK, kf, gkn_bc, cosk[r][t0:t0 + 128, :], sink[r][t0:t0 + 128, :], kbt.t, kbt.b)
                pkt = bank_b(X).rearrange("p (h t) -> p h t", t=128)
                for h in range(8):
                    fw.tr(pkt[0:96, h, :], kbt.t[:, h, :], ident_b.t[:], [kbt.b, ident_b.b], [pbank[X]])
                yield
                fw.cp("act", ktb.t[0:96, :, i * 128:(i + 1) * 128], pkt[0:96, :, :], [pbank[X]], [ktb.b])
                yield

            for r in range(2):
                NK = NKs[r]
                for kb in range(NK // 512):
                    ktb = ktb_r.get()
                    vpb = vpb_r.get()
                    for pair in range(2):
                        gens = [k_tile(r, kb, pair * 2 + par, ktb, vpb, par) for par in range(2)]
                        while gens:
                            for g in list(gens):
                                if next(g, "done") == "done":
                                    gens.remove(g)
                    fw.dma("act", KT[r][:, :, kb * 512:(kb + 1) * 512].rearrange("h d k -> d h k"), ktb.t[:],
                           R=[ktb.b], key=ktb.name)
                    fw.dma("act", VP[r][:, :, kb * 4:(kb + 1) * 4, :].rearrange("h p k v -> p h k v"), vpb.t[:],
                           R=[vpb.b], key=vpb.name)
            fw.barrier()
            ph.close()

        for ph in ([contextlib.ExitStack()] if 'PH2' in phases else []):
            FE = make_fe(ph)
            QK = make_qkn(ph)
            w_in_b = sb(ph, "w_in_b", [128, DC, 4000], BF16)
            wco_b = sb(ph, "wco_b", [128, 4, D], BF16)
            wuq_b = sb(ph, "wuq_b", [128, 2, 768], BF16)
            with contextlib.ExitStack() as ld:
                stg = ring(ld, "stgin", [128, DC, 500], F32, 2)
                for g in range(8):
                    st = stg.get()
                    fw.dma("sp", st.t[:], w_in_d[:, g * 500:(g + 1) * 500].rearrange("(c p) n -> p c n", p=128),
                           W=[st.b], key=st.name)
                    fw.cp("dve" if g % 2 == 0 else "pool", w_in_b.t[:, :, g * 500:(g + 1) * 500], st.t[:],
                          [st.b], [w_in_b.b])
                st2 = sb(ld, "stgco", [128, 4, D], F32)
                fw.dma("sp", st2.t[:], wco_d.rearrange("(c p) n -> p c n", p=128), W=[st2.b], key=st2.name)
                fw.cp("dve", wco_b.t[:], st2.t[:], [st2.b], [wco_b.b])
                st3 = sb(ld, "stguq", [128, 2, 768], F32)
                fw.dma("sp", st3.t[:], wuq_d.rearrange("(c p) n -> p c n", p=128), W=[st3.b], key=st3.name)
                fw.cp("dve", wuq_b.t[:], st3.t[:], [st3.b], [wuq_b.b])
                fw.barrier()
            xr = ring(ph, "o_x", [128, D], F32, 2)
            hTb_r = ring(ph, "o_hTb", [128, DC, 512], BF16, 1)
            hTh_r = ring(ph, "o_hTh", [128, DC, 2], BF16, 2)
            zext_r = ring(ph, "o_zext", [128, 4, 514], F32, 1)
            cg_r = ring(ph, "o_cg", [128, 512], F32, 2)
            cgh = sb(ph, "o_cgh", [128, 4, 2], F32)
            zh = sb(ph, "o_zh", [128, 4, 2], F32)
            y_r = ring(ph, "o_y", [128, 512], F32, 2)
            uT_r = ring(ph, "o_uT", [128, 4, 512], BF16, 2)
            sg_r = ring(ph, "o_sg", [128, 512], F32, 2)
            maT_r = ring(ph, "o_maT", [128, 8, 512], BF16, 1)
            sgbT_r = ring(ph, "o_sgbT", [128, 8, 512], BF16, 1)
            cqn_r = ring(ph, "o_cqn", [128, 256], BF16, 2)
            cqT_r = ring(ph, "o_cqT", [128, 2, 128], BF16, 2)
            qf_r = ring(ph, "o_qf", [128, 8, 96], F32, 2)
            qb_r = ring(ph, "o_qb", [128, 8, 96], BF16, 2)
            qTb_r = ring(ph, "o_qTb", [97, 8, 512], BF16, 1)
            for t_ in qTb_r.tiles:
                fw.ms("pool", t_.t[:], 1.0, [t_.b])
                fw.ts("dve", t_.t[:], t_.t[:], negB.t[0:97, 0:1], None, ALU.mult, None, [t_.b, negB.b], [t_.b])
            for r in range(2):
                for blk in range(NQ // 512):
                    hTb = hTb_r.get()
                    hTh = hTh_r.get()
                    for i in range(4):
                        t0 = 1 + blk * 512 + i * 128
                        xt = xr.get()
                        fw.dma("sp", xt.t[:], xq[r][t0:t0 + 128, :], W=[xt.b], key=xt.name)
                        fe(FE, xt, 128, gmod1[r], shift1[r], hTb.t[:, :, i * 128:(i + 1) * 128], hTb.b, 0)
                    xt = xr.get()
                    fw.dma("sp", xt.t[0:1], xq[r][blk * 512:blk * 512 + 1, :], W=[xt.b], key=xt.name)
                    fw.dma("sp", xt.t[1:2], xq[r][blk * 512 + 513:blk * 512 + 514, :], W=[xt.b], key=xt.name)
                    fe(FE, xt, 2, gmod1[r], shift1[r], hTh.t[:], hTh.b, 0)
                    zext = zext_r.get()
                    for j in range(4):
                        for c in range(DC):
                            fw.mm(psum[:, 1, :], w_in_b.t[:, c, 512 + j * 128:512 + (j + 1) * 128], hTb.t[:, c, :],
                                  c == 0, c == DC - 1, [w_in_b.b, hTb.b], [pbank[1]])
                        cg = cg_r.get()
                        fw.cp("act", cg.t[:], psum[:, 1, :], [pbank[1]], [cg.b])
                        for c in range(DC):
                            fw.mm(psum[:, 2, :], w_in_b.t[:, c, 1024 + j * 128:1024 + (j + 1) * 128], hTb.t[:, c, :],
                                  c == 0, c == DC - 1, [w_in_b.b, hTb.b], [pbank[2]])
                        fw.tt("dve", zext.t[:, j, 1:513], psum[:, 2, :], cg.t[:], ALU.mult, [pbank[2], cg.b], [zext.b])
                    phv = psum[:, 3, 0:16].rearrange("p (j w c) -> p j w c", j=4, w=2)
                    for j in range(4):
                        for w_ in range(2):
                            base = 512 + w_ * 512 + j * 128
                            for c in range(DC):
                                fw.mm(phv[:, j, w_, :], w_in_b.t[:, c, base:base + 128], hTh.t[:, c, :],
                                      c == 0, c == DC - 1, [w_in_b.b, hTh.b], [pbank[3]])
                    fw.cp("act", cgh.t[:], phv[:, :, 0, :], [pbank[3]], [cgh.b])
                    fw.tt("dve", zh.t[:], phv[:, :, 1, :], cgh.t[:], ALU.mult, [pbank[3], cgh.b], [zh.b])
                    if blk == 0:
                        fw.ts("dve", zh.t[:, :, 0:1], zh.t[:, :, 0:1], hm.t[:, r, 0:1], None, ALU.mult, None,
                              [zh.b, hm.b], [zh.b])
                    if blk == NQ // 512 - 1:
                        fw.ts("dve", zh.t[:, :, 1:2], zh.t[:, :, 1:2], hm.t[:, r, 1:2], None, ALU.mult, None,
                              [zh.b, hm.b], [zh.b])
                    fw.cp("dve", zext.t[:, :, 0:1], zh.t[:, :, 0:1], [zh.b], [zext.b])
                    fw.cp("dve", zext.t[:, :, 513:514], zh.t[:, :, 1:2], [zh.b], [zext.b])
                    uT = uT_r.get()
                    for j in range(4):
                        y = y_r.get()
                        fw.ts("dve", y.t[:], zext.t[:, j, 0:512], convw.t[:, j, 0:1], None, ALU.mult, None,
                              [zext.b, convw.b], [y.b])
                        fw.stt(y.t[:], zext.t[:, j, 1:513], convw.t[:, j, 1:2], y.t[:], ALU.mult, ALU.add,
                               [zext.b, convw.b, y.b], [y.b])
                        fw.stt(y.t[:], zext.t[:, j, 2:514], convw.t[:, j, 2:3], y.t[:], ALU.mult, ALU.add,
                               [zext.b, convw.b, y.b], [y.b])
                        for c in range(DC):
                            fw.mm(psum[:, 1 + (j % 2), :], w_in_b.t[:, c, j * 128:(j + 1) * 128], hTb.t[:, c, :],
                                  c == 0, c == DC - 1, [w_in_b.b, hTb.b], [pbank[1 + (j % 2)]])
                        fw.tt("dve", uT.t[:, j, :], psum[:, 1 + (j % 2), :], y.t[:], ALU.mult,
                              [pbank[1 + (j % 2)], y.b], [uT.b])
                    maT = maT_r.get()
                    sgbT = sgbT_r.get()
                    for m in range(8):
                        for j in range(4):
                            fw.mm(psum[:, 4, :], wco_b.t[:, j, m * 128:(m + 1) * 128], uT.t[:, j, :], j == 0, j == 3,
                                  [wco_b.b, uT.b], [pbank[4]])
                        for c in range(DC):
                            fw.mm(psum[:, 5, :], w_in_b.t[:, c, O_GA + m * 128:O_GA + (m + 1) * 128], hTb.t[:, c, :],
                                  c == 0, c == DC - 1, [w_in_b.b, hTb.b], [pbank[5]])
                        sg = sg_r.get()
                        fw.act(sg.t[:], psum[:, 5, :], AF.Sigmoid, [pbank[5]], [sg.b])
                        fw.tt("dve", maT.t[:, m, :], psum[:, 4, :], sg.t[:], ALU.mult, [pbank[4], sg.b], [maT.b])
                        for c in range(DC):
                            fw.mm(psum[:, 6, :], w_in_b.t[:, c, O_GB + m * 128:O_GB + (m + 1) * 128], hTb.t[:, c, :],
                                  c == 0, c == DC - 1, [w_in_b.b, hTb.b], [pbank[6]])
                        fw.act(sgbT.t[:, m, :], psum[:, 6, :], AF.Sigmoid, [pbank[6]], [sgbT.b])
                    sl = slice(blk * 512, (blk + 1) * 512)
                    fw.dma("act", MA[r][:, :, sl].rearrange("m p t -> p m t"), maT.t[:], R=[maT.b], key=maT.name)
                    fw.dma("act", SGB[r][:, :, sl].rearrange("m p t -> p m t"), sgbT.t[:], R=[sgbT.b], key=sgbT.name)
                    qTb = qTb_r.get()
                    for i in range(4):
                        for c in range(DC):
                            fw.mm(psum[:, 7, 0:256], hTb.t[:, c, i * 128:(i + 1) * 128], w_in_b.t[:, c, O_CQ:O_CQ + 256],
                                  c == 0, c == DC - 1, [hTb.b, w_in_b.b], [pbank[7]])
                        rstd = rms_small(FE, psum[:, 7, 0:256], [pbank[7]], 256)
                        cqn = cqn_r.get()
                        fw.stt(cqn.t[:], psum[:, 7, 0:256], rstd.t[:], gq_bc.t[:], ALU.mult, ALU.mult,
                               [pbank[7], rstd.b, gq_bc.b], [cqn.b])
                        ptq = bank_b(3)[:, 0:256].rearrange("p (c t) -> p c t", t=128)
                        for c2 in range(2):
                            fw.tr(ptq[:, c2, :], cqn.t[:, c2 * 128:(c2 + 1) * 128], ident_b.t[:],
                                  [cqn.b, ident_b.b], [pbank[3]])
                        cqT = cqT_r.get()
                        fw.cp("dve", cqT.t[:], ptq, [pbank[3]], [cqT.b])
                        for c2 in range(2):
                            fw.mm(psum[:, 1, :], cqT.t[:, c2, :], wuq_b.t[:, c2, 0:512], c2 == 0, c2 == 1,
                                  [cqT.b, wuq_b.b], [pbank[1]])
                        for c2 in range(2):
                            fw.mm(psum[:, 2, 0:256], cqT.t[:, c2, :], wuq_b.t[:, c2, 512:768], c2 == 0, c2 == 1,
                                  [cqT.b, wuq_b.b], [pbank[2]])
                        qf = qf_r.get()
                        qfv = qf.t[:].rearrange("p h d -> p (h d)")
                        fw.cp("act", qfv[:, 0:512], psum[:, 1, :], [pbank[1]], [qf.b])
                        fw.cp("act", qfv[:, 512:768], psum[:, 2, 0:256], [pbank[2]], [qf.b])
                        qb = qb_r.get()
                        q0 = blk * 512 + i * 128
                        qknorm_rope(QK, qf, gqs_bc, cosq[r][q0:q0 + 128, :], sinq[r][q0:q0 + 128, :], qb.t, qb.b)
                        pkt = bank_b(3).rearrange("p (h t) -> p h t", t=128)
                        for h in range(8):
                            fw.tr(pkt[0:96, h, :], qb.t[:, h, :], ident_b.t[:], [qb.b, ident_b.b], [pbank[3]])
                        fw.cp("act", qTb.t[0:96, :, i * 128:(i + 1) * 128], pkt[0:96, :, :], [pbank[3]], [qTb.b])
                    fw.dma("act", QT[r][:, :, sl].rearrange("h d k -> d h k"), qTb.t[:], R=[qTb.b], key=qTb.name)
            fw.barrier()
            ph.close()

        for ph in ([contextlib.ExitStack()] if 'PH3' in phases else []):
            NKmax = max(NKs)
            kt_r = ring(ph, "a_kt", [97, NKmax], BF16, 2)
            vp_r = ring(ph, "a_vp", [128, NKmax // 128, 66], BF16, 2)
            qt_r = ring(ph, "a_qt", [97, NQ], BF16, 2)
            pT_r = ring(ph, "a_pT", [128, 512], BF16, 4)
            osb_r = ring(ph, "a_osb", [65, 512], F32, 2)
            rl_r = ring(ph, "a_rl", [64, 512], F32, 2)
            obh_r = ring(ph, "a_obh", [64, NQ], BF16, 2)
            ps_box = [0]
            po_i = 0
            for r in range(2):
                NK = NKs[r]
                nkt = NK // 128
                for h in range(8):
                    ktt = kt_r.get()
                    vpt = vp_r.get()
                    qtt = qt_r.get()
                    fw.dma("sp", ktt.t[:, 0:NK], KT[r][h], W=[ktt.b], key=ktt.name)
                    fw.dma("sp", vpt.t[:, 0:nkt, :], VP[r][h], W=[vpt.b], key=vpt.name)
                    fw.dma("sp", qtt.t[:], QT[r][h], W=[qtt.b], key=qtt.name)
                    obh = obh_r.get()
                    for qc in range(NQ // 512):
                        pob = 4 + (po_i % 2)
                        po_i += 1
                        LOOK = 2
                        sbanks = {}

                        def issue_S(j, qc=qc, ktt=ktt, qtt=qtt):
                            nonlocal_ps = ps_box[0] % 4
                            ps_box[0] += 1
                            sbanks[j] = nonlocal_ps
                            fw.mm(psum[:, nonlocal_ps, :], ktt.t[:, j * 128:(j + 1) * 128], qtt.t[:, qc * 512:(qc + 1) * 512],
                                  True, True, [ktt.b, qtt.b], [pbank[nonlocal_ps]])
                        for j in range(min(LOOK, nkt)):
                            issue_S(j)
                        for kt in range(nkt):
                            if kt + LOOK < nkt:
                                issue_S(kt + LOOK)
                            psb = sbanks.pop(kt)
                            pT = pT_r.get()
                            fw.act(pT.t[:], psum[:, psb, :], AF.Exp, [pbank[psb]], [pT.b])
                            fw.mm(psum[0:65, pob, :], vpt.t[:, kt, 0:65], pT.t[:], kt == 0, kt == nkt - 1,
                                  [vpt.b, pT.b], [pbank[pob]])
                        osb = osb_r.get()
                        fw.cp("dve", osb.t[:], psum[0:65, pob, :], [pbank[pob]], [osb.b])
                        fw.mm(psum[0:64, 6, :], sel65.t[:], osb.t[:], True, True, [sel65.b, osb.b], [pbank[6]])
                        rl = rl_r.get()
                        fw.recip(rl.t[:], psum[0:64, 6, :], [pbank[6]], [rl.b])
                        fw.tt("pool", obh.t[:, qc * 512:(qc + 1) * 512], osb.t[0:64, :], rl.t[:], ALU.mult,
                              [osb.b, rl.b], [obh.b])
                    fw.dma("act", OB[r][h], obh.t[:], R=[obh.b], key=obh.name)
            fw.barrier()
            ph.close()

        for ph in ([contextlib.ExitStack()] if 'PH4' in phases else []):
            wao_b = sb(ph, "wao_b", [64, 8, D], BF16)
            woutg = sb(ph, "woutg", [128, DC, D], BF16)
            with contextlib.ExitStack() as ld:
                st = sb(ld, "stgao", [64, 8, D], F32)
                fw.dma("sp", st.t[:], wao_d.rearrange("(h p) n -> p h n", p=64), W=[st.b], key=st.name)
                fw.cp("dve", wao_b.t[:], st.t[:], [st.b], [wao_b.b])
                fw.barrier()
            stw = sb(ph, "stgwo", [128, DC, D], F32)
            fw.dma("sp", stw.t[:], wout_d.rearrange("(c p) n -> p c n", p=128), W=[stw.b], key=stw.name)
            ma_r = ring(ph, "m_ma", [128, 8, 512], BF16, 2)
            sgb_r = ring(ph, "m_sgb", [128, 8, 512], BF16, 2)
            ob_r = ring(ph, "m_ob", [64, 8, 512], BF16, 2)
            t1_r = ring(ph, "m_t1", [128, 512], F32, 2)
            mg_r = ring(ph, "m_mg", [128, 8, 512], BF16, 2)
            xr = ring(ph, "m_x", [128, D], F32, 3)
            y1_r = ring(ph, "m_y1", [128, D], F32, 3)
            g1bc = sb(ph, "g1bc", [128, D], F32)
            for r in range(2):
                fw.dma("sp", g1bc.t[:], GATE[r, 0], W=[g1bc.b], key=g1bc.name)
                for c in range(DC):
                    fw.tt("dve" if c % 2 == 0 else "pool", woutg.t[:, c, :], stw.t[:, c, :], g1bc.t[:], ALU.mult,
                          [stw.b, g1bc.b], [woutg.b])
                for blk in range(NQ // 512):
                    sl = slice(blk * 512, (blk + 1) * 512)
                    ma = ma_r.get()
                    sgb = sgb_r.get()
                    ob = ob_r.get()
                    fw.dma("sp", ma.t[:], MA[r][:, :, sl].rearrange("m p t -> p m t"), W=[ma.b], key=ma.name)
                    fw.dma("sp", sgb.t[:], SGB[r][:, :, sl].rearrange("m p t -> p m t"), W=[sgb.b], key=sgb.name)
                    fw.dma("sp", ob.t[:], OB[r][:, :, sl].rearrange("h p t -> p h t"), W=[ob.b], key=ob.name)
                    mg = mg_r.get()
                    for m in range(8):
                        bk = m % 2
                        for h in range(8):
                            fw.mm(psum[:, bk, :], wao_b.t[:, h, m * 128:(m + 1) * 128], ob.t[:, h, :], h == 0, h == 7,
                                  [wao_b.b, ob.b], [pbank[bk]])
                        t1 = t1_r.get()
                        fw.tt("dve", t1.t[:], psum[:, bk, :], sgb.t[:, m, :], ALU.mult, [pbank[bk], sgb.b], [t1.b])
                        fw.tt("pool", mg.t[:, m, :], t1.t[:], ma.t[:, m, :], ALU.add, [t1.b, ma.b], [mg.b])
                    for i in range(4):
                        t0 = blk * 512 + i * 128
                        xt = xr.get()
                        fw.dma("sp", xt.t[:], xq[r][1 + t0:1 + t0 + 128, :], W=[xt.b], key=xt.name)
                        pb0 = 2 + 2 * (i % 2)
                        for half in range(2):
                            for k in range(DC):
                                fw.mm(psum[:, pb0 + half, :], mg.t[:, k, i * 128:(i + 1) * 128],
                                      woutg.t[:, k, half * 512:(half + 1) * 512], k == 0, k == DC - 1,
                                      [mg.b, woutg.b], [pbank[pb0 + half]])
                        y1 = y1_r.get()
                        for half in range(2):
                            hs = slice(half * 512, (half + 1) * 512)
                            fw.tt("dve", y1.t[:, hs], psum[:, pb0 + half, :], xt.t[:, hs], ALU.add,
                                  [pbank[pb0 + half], xt.b], [y1.b])
                        fw.dma("act", Y1[r][t0:t0 + 128, :], y1.t[:], R=[y1.b], key=y1.name)
            fw.barrier()
            ph.close()

        TB = 256
        for ph in ([contextlib.ExitStack()] if 'PH5' in phases else []):
            FE = make_fe(ph, 1)
            wq_b = sb(ph, "wq_b", [128, DC, D], BF16)
            k12_b = sb(ph, "k12_b", [128, 8, 128], BF16)
            with contextlib.ExitStack() as ld:
                st = sb(ld, "stgwq", [128, DC, D], F32)
                fw.dma("sp", st.t[:], wq_d.rearrange("(c p) n -> p c n", p=128), W=[st.b], key=st.name)
                fw.cp("dve", wq_b.t[:], st.t[:], [st.b], [wq_b.b])
                st2 = sb(ld, "stgk12", [128, 8, 128], F32)
                load(st2, k12_d)
                fw.cp("dve", k12_b.t[:], st2.t[:], [st2.b], [k12_b.b])
                fw.barrier()
            Wsb = sb(ph, "Wsb", [128, 128, TB], BF16)
            y1t_r = ring(ph, "e_y1", [128, D], F32, 4)
            h2T_r = ring(ph, "e_h2T", [128, DC, TB], BF16, 2)
            qT_r = ring(ph, "e_qT", [128, 8, TB], BF16, 1)
            s12 = sb(ph, "e_s12", [128, 8, 2, 128], F32)
            s12w = sb(ph, "e_s12w", [128, 8, 2, 128], F32)
            v12 = sb(ph, "e_v12", [128, 8, 2, 16], F32)
            i12 = sb(ph, "e_i12", [128, 8, 2, 16], U32)
            i12f = sb(ph, "e_i12f", [128, 8, 2, 16], F32)
            comb = sb(ph, "e_comb", [128, 8, 256], F32)
            combw = sb(ph, "e_combw", [128, 8, 256], F32)
            sc = sb(ph, "e_sc", [128, 8, 16], F32)
            pos = sb(ph, "e_pos", [128, 8, 16], U32)
            posa = sb(ph, "e_posa", [128, 8, 16], U32)
            posb = sb(ph, "e_posb", [128, 8, 16], U32)
            iaf = sb(ph, "e_iaf", [128, 8, 16], F32)
            ibf = sb(ph, "e_ibf", [128, 8, 16], F32)
            eq = sb(ph, "e_eq", [128, 4, 16, 16], F32)
            hlw = sb(ph, "e_hlw", [128, 3, 128], F32)
            ex = sb(ph, "e_ex", [128, 8, 16], F32)
            zz = sb(ph, "e_zz", [128, 8], F32)
            jT_r = ring(ph, "e_jT", [128, 3, TB], F32, 1)
            ohl_r = ring(ph, "e_ohl", [128, 8, 128], BF16, 2)
            ohh_r = ring(ph, "e_ohh", [128, 8, 128], BF16, 2)
            ut_r = ring(ph, "e_ut", [128, DC, 128], BF16, 2)
            vt_r = ring(ph, "e_vt", [128, D], BF16, 3)
            g1_r = ring(ph, "e_g1", [128, TB], BF16, 2)
            G_r = ring(ph, "e_G", [128, TB], BF16, 2)
            yo_r = ring(ph, "e_yo", [128, D], F32, 1)
            g2bc = sb(ph, "g2bc", [128, D], F32)
            hb = [(fw.buf("v12_%d" % i), fw.buf("i12_%d" % i), fw.buf("s12w_%d" % i)) for i in range(16)]
            hb2 = [(fw.buf("sc_%d" % i), fw.buf("pos_%d" % i), fw.buf("combw_%d" % i)) for i in range(8)]
            pw_box = [0]
            pa_box = [0]

            def prep_topk(r, blk, h2T, qT, jT, y1ts):
                for i in range(TB // 128):
                    t0 = blk * TB + i * 128
                    y1t = y1t_r.get()
                    fw.dma("sp", y1t.t[:], Y1[r][t0:t0 + 128, :], W=[y1t.b], key=y1t.name)
                    y1ts.append(y1t)
                    fe(FE, y1t, 128, gmod2[r], shift2[r], h2T.t[:, :, i * 128:(i + 1) * 128], h2T.b, 6)
                    yield
                for h in range(8):
                    bk = 6 + (h % 2)
                    for c in range(DC):
                        fw.mm(psum[:, bk, 0:TB], wq_b.t[:, c, h * 128:(h + 1) * 128], h2T.t[:, c, :],
                              c == 0, c == DC - 1, [wq_b.b, h2T.b], [pbank[bk]])
                    fw.cp("act", qT.t[:, h, :], psum[:, bk, 0:TB], [pbank[bk]], [qT.b])
                    if h % 2 == 1:
                        yield
                for i in range(TB // 128):
                    tsl = slice(i * 128, (i + 1) * 128)
                    pssb = [psum[:, 6 + a_, :].rearrange("p (h n) -> p h n", n=128) for a_ in range(2)]
                    for hg in range(2):
                        for hh in range(4):
                            h = hg * 4 + hh
                            fw.mm(pssb[0][:, hh, :], qT.t[0:64, h, tsl], k12_b.t[0:64, h, :], True, True,
                                  [qT.b, k12_b.b], [pbank[6]])
                            fw.mm(pssb[1][:, hh, :], qT.t[64:128, h, tsl], k12_b.t[64:128, h, :], True, True,
                                  [qT.b, k12_b.b], [pbank[7]])
                        for a_ in range(2):
                            fw.cp("act", s12.t[:, hg * 4:(hg + 1) * 4, a_, :], pssb[a_], [pbank[6 + a_]], [s12.b])
                        yield
                    for stage in range(5):
                        for ha in range(16):
                            h, a = ha // 2, ha % 2
                            bv, bi, bw = hb[ha]
                            if stage == 0:
                                fw.op("dve", lambda e, h=h, a=a: e.max(out=v12.t[:, h, a, 0:8], in_=s12.t[:, h, a, :]),
                                      [s12.b], [bv])
                            elif stage == 1:
                                fw.op("dve", lambda e, h=h, a=a: e.max_index(out=i12.t[:, h, a, 0:8], in_max=v12.t[:, h, a, 0:8],
                                                                             in_values=s12.t[:, h, a, :]), [s12.b, bv], [bi])
                            elif stage == 2:
                                fw.op("dve", lambda e, h=h, a=a: e.match_replace(out=s12w.t[:, h, a, :], in_to_replace=v12.t[:, h, a, 0:8],
                                                                                 in_values=s12.t[:, h, a, :], imm_value=NEG),
                                      [s12.b, bv], [bw])
                            elif stage == 3:
                                fw.op("dve", lambda e, h=h, a=a: e.max(out=v12.t[:, h, a, 8:16], in_=s12w.t[:, h, a, :]),
                                      [bw], [bv])
                            else:
                                fw.op("dve", lambda e, h=h, a=a: e.max_index(out=i12.t[:, h, a, 8:16], in_max=v12.t[:, h, a, 8:16],
                                                                             in_values=s12w.t[:, h, a, :]), [bw, bv], [bi])
                            if ha % 8 == 7:
                                yield
                    allv = [x[0] for x in hb]
                    alli = [x[1] for x in hb]
                    combv = comb.t[:].rearrange("p h (a b) -> p h a b", b=16)
                    fw.tt("dve", combv, bcl(v12.t[:, :, 0, :], 16),
                          v12.t[:, :, 1, :][:, :, None, :].to_broadcast([128, 8, 16, 16]), ALU.add,
                          allv, [comb.b])
                    yield
                    for stage in range(5):
                        for h in range(8):
                            bs, bp, bw = hb2[h]
                            if stage == 0:
                                fw.op("dve", lambda e, h=h: e.max(out=sc.t[:, h, 0:8], in_=comb.t[:, h, :]), [comb.b], [bs])
                            elif stage == 1:
                                fw.op("dve", lambda e, h=h: e.max_index(out=pos.t[:, h, 0:8], in_max=sc.t[:, h, 0:8],
                                                                        in_values=comb.t[:, h, :]), [comb.b, bs], [bp])
                            elif stage == 2:
                                fw.op("dve", lambda e, h=h: e.match_replace(out=combw.t[:, h, :], in_to_replace=sc.t[:, h, 0:8],
                                                                            in_values=comb.t[:, h, :], imm_value=NEG),
                                      [comb.b, bs], [bw])
                            elif stage == 3:
                                fw.op("dve", lambda e, h=h: e.max(out=sc.t[:, h, 8:16], in_=combw.t[:, h, :]), [bw], [bs])
                            else:
                                fw.op("dve", lambda e, h=h: e.max_index(out=pos.t[:, h, 8:16], in_max=sc.t[:, h, 8:16],
                                                                        in_values=combw.t[:, h, :]), [bw, bs], [bp])
                        yield
                    alls = [x[0] for x in hb2]
                    allp = [x[1] for x in hb2]
                    fw.op("dve", lambda e: e.tensor_single_scalar(out=posa.t[:], in_=pos.t[:], scalar=4,
                                                                  op=ALU.logical_shift_right), allp, [posa.b])
                    fw.op("dve", lambda e: e.tensor_single_scalar(out=posb.t[:], in_=pos.t[:], scalar=15,
                                                                  op=ALU.bitwise_and), allp, [posb.b])
                    fw.cp("dve", iaf.t[:], posa.t[:], [posa.b], [iaf.b])
                    fw.cp("dve", ibf.t[:], posb.t[:], [posb.b], [ibf.b])
                    fw.cp("dve", i12f.t[:], i12.t[:], alli, [i12f.b])
                    yield
                    io16 = iota_f.t[:, 0:16][:, None, None, :].to_broadcast([128, 4, 16, 16])
                    for which, sel_f in ((0, iaf), (1, ibf)):
                        for hq in range(2):
                            hs = slice(hq * 4, hq * 4 + 4)
                            fw.tt("dve", eq.t[:], io16, bcl(sel_f.t[:, hs, :], 16), ALU.is_equal, [iota_f.b, sel_f.b], [eq.b])
                            fw.tt("dve", eq.t[:], eq.t[:],
                                  i12f.t[:, hs, which, :][:, :, None, :].to_broadcast([128, 4, 16, 16]), ALU.mult,
                                  [eq.b, i12f.b], [eq.b])
                            fw.red(hlw.t[:, which, hq * 64:(hq + 1) * 64].rearrange("p (h k) -> p h k", k=16), eq.t[:], ALU.add,
                                   [eq.b], [hlw.b])
                            yield
                    fw.tt("dve", ex.t[:], sc.t[:], bcl(sc.t[:, :, 0], 16), ALU.subtract, alls, [ex.b])
                    fw.act(ex.t[:], ex.t[:], AF.Exp, [ex.b], [ex.b])
                    fw.red(zz.t[:], ex.t[:], ALU.add, [ex.b], [zz.b])
                    fw.recip(zz.t[:], zz.t[:], [zz.b], [zz.b])
                    fw.tt("dve", hlw.t[:, 2, :].rearrange("p (h k) -> p h k", k=16), ex.t[:], bcl(zz.t[:], 16),
                          ALU.mult, [ex.b, zz.b], [hlw.b])
                    pj = psum[:, 6, 0:384].rearrange("p (w t) -> p w t", t=128)
                    for w_ in range(3):
                        fw.tr(pj[:, w_, :], hlw.t[:, w_, :], ident_f.t[:], [hlw.b, ident_f.b], [pbank[6]])
                    fw.cp("act", jT.t[:, :, tsl], pj, [pbank[6]], [jT.b])
                    yield

            def w_build(jT):
                for sub in range(TB // 8):
                    ohl = ohl_r.get()
                    ohh = ohh_r.get()
                    for u in range(8):
                        t = sub * 8 + u
                        fw.ts("dve", ohl.t[:, u, :], iota_f.t[:], jT.t[:, 1, t:t + 1], None, ALU.is_equal, None,
                              [iota_f.b, jT.b], [ohl.b])
                        fw.ts("dve", ohh.t[:, u, :], iota_f.t[:], jT.t[:, 0, t:t + 1], jT.t[:, 2, t:t + 1],
                              ALU.is_equal, ALU.mult, [iota_f.b, jT.b], [ohh.b])
                    for g in range(2):
                        bk = 6 + (pw_box[0] % 2)
                        pw_box[0] += 1
                        pwv = psum[:, bk, :].rearrange("p (u c) -> p u c", c=128)
                        for u in range(4):
                            fw.mm(pwv[:, u, :], ohl.t[:, g * 4 + u, :], ohh.t[:, g * 4 + u, :], True, True,
                                  [ohl.b, ohh.b], [pbank[bk]])
                        t0 = sub * 8 + g * 4
                        fw.cp("act", Wsb.t[:, :, t0:t0 + 4].rearrange("p c t -> p t c"), pwv, [pbank[bk]], [Wsb.b])

            def chunk_loop(h2T):
                ainfo = {}

                def issue_A(c):
                    ut = ut_r.get()
                    vt = vt_r.get()
                    fw.dma("sp", ut.t[:], UTb[c].rearrange("p (dc e) -> p dc e", e=128), W=[ut.b], key=ut.name)
                    fw.dma("sp", vt.t[:], Vb[c * 128:(c + 1) * 128, :], W=[vt.b], key=vt.name)
                    bk = 4 + (pa_box[0] % 2)
                    pa_box[0] += 1
                    for dc in range(DC):
                        fw.mm(psum[:, bk, 0:TB], ut.t[:, dc, :], h2T.t[:, dc, :], dc == 0, dc == DC - 1,
                              [ut.b, h2T.b], [pbank[bk]])
                    ainfo[c] = (bk, vt)
                issue_A(0)
                for c in range(NCH):
                    if c + 1 < NCH:
                        issue_A(c + 1)
                    bk, vt = ainfo.pop(c)
                    g1 = g1_r.get()
                    fw.act(g1.t[:], psum[:, bk, 0:TB], AF.Gelu_apprx_tanh, [pbank[bk]], [g1.b])
                    G = G_r.get()
                    fw.tt("dve", G.t[:], g1.t[:], Wsb.t[:, c, :], ALU.mult, [g1.b, Wsb.b], [G.b])
                    for i in range(TB // 128):
                        for half in range(2):
                            ab = i * 2 + half
                            fw.mm(psum[:, ab, :], G.t[:, i * 128:(i + 1) * 128], vt.t[:, half * 512:(half + 1) * 512],
                                  c == 0, c == NCH - 1, [G.b, vt.b], [pbank[ab]])
                    yield

            def final(r, blk, y1ts):
                if blk == 0:
                    fw.dma("sp", g2bc.t[:], GATE[r, 1], W=[g2bc.b], key=g2bc.name)
                for i in range(TB // 128):
                    t0 = blk * TB + i * 128
                    yo = yo_r.get()
                    for half in range(2):
                        hs = slice(half * 512, (half + 1) * 512)
                        fw.tt("dve", yo.t[:, hs], psum[:, 2 * i + half, :], g2bc.t[:, hs],
                              ALU.mult, [pbank[2 * i + half], g2bc.b], [yo.b])
                    fw.tt("pool", yo.t[:], yo.t[:], y1ts[i].t[:], ALU.add, [yo.b, y1ts[i].b], [yo.b])
                    fw.dma("act", out_d[r][t0:t0 + 128, :], yo.t[:], R=[yo.b], key=yo.name)

            blocks = [(r, blk) for r in range(2) for blk in range(NQ // TB)]
            jT = jT_r.get()
            qT = qT_r.get()

            def new_state():
                return {"h2T": h2T_r.get(), "y1ts": []}
            cur = new_state()
            for _ in prep_topk(blocks[0][0], blocks[0][1], cur["h2T"], qT, jT, cur["y1ts"]):
                pass
            for n, (r, blk) in enumerate(blocks):
                w_build(jT)
                nxt = None
                gen = None
                if n + 1 < len(blocks):
                    nxt = new_state()
                    gen = prep_topk(blocks[n + 1][0], blocks[n + 1][1], nxt["h2T"], qT, jT, nxt["y1ts"])
                for _ in chunk_loop(cur["h2T"]):
                    if gen is not None:
                        if next(gen, "done") == "done":
                            gen = None
                if gen is not None:
                    for _ in gen:
                        pass
                final(r, blk, cur["y1ts"])
                cur = nxt
            fw.barrier()
            ph.close()
        fw.finalize()
    return nc, fw


def _rope_tables(pos):
    pos = pos.astype(np.float32)
    inv = (np.float32(10000.0) ** (-np.arange(0, 32, 2, dtype=np.float32) / np.float32(32))).astype(np.float32)
    ang = (pos[:, None] * inv[None, :]).astype(np.float32)
    return np.cos(ang).astype(np.float32), np.sin(ang).astype(np.float32)


def prep_inputs(inp, NQ):
    f = lambda a: np.ascontiguousarray(np.asarray(a, dtype=np.float32))
    xp = f(inp["x_prompt"])
    xs = f(inp["x_sample"])
    NKP = xp.shape[1]
    NKS = xs.shape[1]
    assert NKP == 4 * NQ and NKS == 2 * NQ and xp.shape[0] == 2 and xs.shape[0] == 4

    def colT(v, nch):
        return np.ascontiguousarray(v.reshape(nch, 128).T)

    def rep(v):
        return np.ascontiguousarray(np.broadcast_to(v[None, :], (128, v.shape[0])))

    k1 = f(inp["peer_k1"])[0]
    k2 = f(inp["peer_k2"])[0]
    k12T = np.concatenate([k1.transpose(2, 0, 1), k2.transpose(2, 0, 1)], axis=0)
    U = f(inp["peer_u"])[0]
    UT = np.ascontiguousarray(U.reshape(NCH, 128, DC, 128).transpose(0, 3, 2, 1)).reshape(NCH, 128, D)
    convw = f(inp["conv_w"])[0]
    convw_l = np.ascontiguousarray(convw.reshape(3, 4, 128).transpose(2, 1, 0))
    selA = np.zeros((2, 2, 128), np.float32)
    selA[0, 0, :] = 1.0
    selA[1, 1, :] = 1.0
    sel65 = np.zeros((65, 64), np.float32)
    sel65[64, :] = 1.0
    shared = {
        "w_ada": f(inp["w_ada"])[0],
        "b_ada2": np.ascontiguousarray(np.broadcast_to(f(inp["b_ada"])[0][None, :], (2, 6 * D))),
        "g1T": colT(f(inp["g_norm1"])[0], DC),
        "g2T": colT(f(inp["g_norm2"])[0], DC),
        "w_in": f(inp["w_in"])[0],
        "convw": convw_l,
        "w_conv_out": f(inp["w_conv_out"])[0],
        "gq_bc": rep(f(inp["g_q_lora"])[0]),
        "w_uq": f(inp["w_uq"])[0],
        "gkv_bc": rep(f(inp["g_kv_lora"])[0]),
        "w_ukv": f(inp["w_ukv"])[0],
        "gqn_bc": rep(f(inp["g_qnorm"])[0]),
        "gkn_bc": rep(f(inp["g_knorm"])[0]),
        "w_attn_out": f(inp["w_attn_out"])[0],
        "w_out": f(inp["w_out"])[0],
        "peer_wq": f(inp["peer_wq"])[0],
        "k12T": np.ascontiguousarray(k12T),
        "UT": UT[:1] if TINY_TABLES else UT,
        "peer_v": f(inp["peer_v"])[0][:128] if TINY_TABLES else f(inp["peer_v"])[0],
        "ident": np.eye(128, dtype=np.float32),
        "iota": np.ascontiguousarray(np.broadcast_to(np.arange(128, dtype=np.float32)[None, :], (128, 128))),
        "selA": selA,
        "sel65": sel65,
    }
    cp = f(inp["c_prompt"])
    cs = f(inp["c_sample"])
    ckp, skp = _rope_tables(np.arange(NKP))
    cks, sks = _rope_tables(np.arange(NKS))
    maps = []
    for i in range(8):
        pb, pq = i // 4, i % 4
        sb_, sh = i // 2, i % 2
        m = dict(shared)
        m["xk_p"] = xp[pb]
        m["xk_s"] = xs[sb_]
        hmask = np.zeros((128, 2, 2), np.float32)
        for r, (xfull, q0, NK) in enumerate(((xp[pb], pq * NQ, NKP), (xs[sb_], sh * NQ, NKS))):
            ext = np.zeros((NQ + 2, D), np.float32)
            ext[1:NQ + 1] = xfull[q0:q0 + NQ]
            if q0 > 0:
                ext[0] = xfull[q0 - 1]
                hmask[:, r, 0] = 1.0
            if q0 + NQ < NK:
                ext[NQ + 1] = xfull[q0 + NQ]
                hmask[:, r, 1] = 1.0
            m["xq_p" if r == 0 else "xq_s"] = ext
        m["hmask"] = hmask
        m["cT2"] = np.ascontiguousarray(np.stack([colT(cp[pb], DC), colT(cs[sb_], DC)], axis=-1))
        m["cosk_p"], m["sink_p"] = ckp, skp
        m["cosk_s"], m["sink_s"] = cks, sks
        m["cosq_p"] = np.ascontiguousarray(ckp[pq * NQ:(pq + 1) * NQ])
        m["sinq_p"] = np.ascontiguousarray(skp[pq * NQ:(pq + 1) * NQ])
        m["cosq_s"] = np.ascontiguousarray(cks[sh * NQ:(sh + 1) * NQ])
        m["sinq_s"] = np.ascontiguousarray(sks[sh * NQ:(sh + 1) * NQ])
        maps.append(m)
    return maps, NKP, NKS


def run(inputs, NQ):
    maps, NKP, NKS = prep_inputs(inputs, NQ)
    nc, fw = build(NQ, NKP, NKS)
    res = run_bass_kernel_spmd(nc, maps, core_ids=list(range(8)))
    yp = np.zeros((2, NKP, D), np.float32)
    ys = np.zeros((4, NKS, D), np.float32)
    for i in range(8):
        pb, pq = i // 4, i % 4
        sb_, sh = i // 2, i % 2
        yp[pb, pq * NQ:(pq + 1) * NQ] = res.results[i]["out_p"]
        ys[sb_, sh * NQ:(sh + 1) * NQ] = res.results[i]["out_s"]
    return yp, ys


def kernel(**inputs):
    return run(inputs, 4096)
```

```python
import contextlib
import numpy as np
import concourse.bass as bass
import concourse.mybir as mybir
from concourse.bass_utils import run_bass_kernel_spmd

F32 = mybir.dt.float32
BF16 = mybir.dt.bfloat16
U32 = mybir.dt.uint32
AF = mybir.ActivationFunctionType
ALU = mybir.AluOpType
AX = mybir.AxisListType

D = 1024
DC = 8
EPS = 1e-6
NE = 16384
NCH = 128
O_CQ = 1536
O_CKV = 1792
O_GA = 1952
O_GB = 2976
NEG = -1.0e30
TINY_TABLES = False


class _Stop(Exception):
    pass


STOPPED = [False]


def chk(n):
    import os
    if int(os.environ.get("K_STOP", "100000")) == n:
        if n >= 100:
            STOPPED[0] = True
        else:
            raise _Stop()


class Buf:
    __slots__ = ("name", "last_w", "readers", "excl")

    def __init__(self, name, excl=False):
        self.name = name
        self.last_w = None
        self.readers = []
        self.excl = excl


class T:
    __slots__ = ("t", "b", "name")

    def __init__(self, t, b, name):
        self.t = t
        self.b = b
        self.name = name


class Ring:
    def __init__(self, tiles):
        self.tiles = tiles
        self.i = 0

    def get(self):
        t = self.tiles[self.i % len(self.tiles)]
        self.i += 1
        return t


class FW:
    def __init__(self, nc):
        self.nc = nc
        self.names = ["sp", "pe", "dve", "act", "pool"]
        self.sem = {k: nc.alloc_semaphore(name="s_" + k) for k in self.names}
        self.cnt = {k: 0 for k in self.names}
        self.waited = {k: {} for k in self.names}
        self.dsems = {}
        self.dcnt = {}
        self.semobj = dict(self.sem)
        self.prog = {k: [] for k in self.names}
        self.bufs = []
        self.n_inst = 0

    def buf(self, name, excl=False):
        b = Buf(name, excl)
        self.bufs.append(b)
        return b

    def _needs(self, reads, writes):
        needs = {}

        def add(ev):
            if ev is None:
                return
            k = ev[0]
            if k not in needs or needs[k] < ev[1]:
                needs[k] = ev[1]
        for b in reads:
            add(b.last_w)
        for b in writes:
            add(b.last_w)
            for r in b.readers:
                add(r)
        return needs

    def _do_waits(self, eng, needs):
        w = self.waited[eng]
        for k, val in needs.items():
            if k == eng:
                if eng in ("pe", "sp"):
                    continue
            if w.get(k, 0) >= val:
                continue
            self.prog[eng].append(("w", self.semobj[k], val))
            w[k] = val

    def _record(self, ev, reads, writes):
        for b in reads:
            rs = b.readers
            for i, r in enumerate(rs):
                if r[0] == ev[0]:
                    rs[i] = ev
                    break
            else:
                rs.append(ev)
        for b in writes:
            b.last_w = ev
            b.readers = []

    def op(self, eng, fn, R=(), W=()):
        if STOPPED[0]:
            return
        if any(b.excl for b in R):
            W = list(W) + [b for b in R if b.excl]
            R = [b for b in R if not b.excl]
        needs = self._needs(R, W)
        self._do_waits(eng, needs)
        self.cnt[eng] += 1
        self.prog[eng].append(("i", fn, self.sem[eng], 1))
        ev = (eng, self.cnt[eng])
        self._record(ev, R, W)
        self.n_inst += 1

    def dma(self, q, out, in_, R=(), W=(), key=None):
        if STOPPED[0]:
            return
        if key not in self.dsems:
            s = self.nc.alloc_semaphore(name="d_" + str(key))
            self.dsems[key] = s
            self.semobj[("d", key)] = s
            self.dcnt[key] = 0
        needs = self._needs(R, W)
        self._do_waits(q, needs)
        self.prog[q].append(("i", (lambda e, out=out, in_=in_: e.dma_start(out=out, in_=in_)),
                             self.dsems[key], 16))
        self.dcnt[key] += 16
        ev = (("d", key), self.dcnt[key])
        self._record(ev, R, W)
        self.n_inst += 1

    def barrier(self):
        allk = [(k, self.cnt[k]) for k in self.names] + [(("d", k), v) for k, v in self.dcnt.items()]
        for e in self.names:
            for k, v in allk:
                if k == e or v == 0:
                    continue
                if self.waited[e].get(k, 0) >= v:
                    continue
                self.prog[e].append(("w", self.semobj[k], v))
                self.waited[e][k] = v
        for b in self.bufs:
            b.last_w = None
            b.readers = []

    def finalize(self):
        nc = self.nc
        prog = self.prog

        def run(e, lst):
            for t in lst:
                if t[0] == "w":
                    e.wait_ge(t[1], t[2])
                else:
                    t[1](e).then_inc(t[2], t[3])
        with nc.Block() as block:
            @block.sync
            def _(e):
                run(e, prog["sp"])

            @block.tensor
            def _(e):
                run(e, prog["pe"])

            @block.vector
            def _(e):
                run(e, prog["dve"])

            @block.scalar
            def _(e):
                run(e, prog["act"])

            @block.gpsimd
            def _(e):
                run(e, prog["pool"])

    def mm(self, out, lhsT, rhs, start, stop, R, W):
        self.op("pe", lambda e: e.matmul(out, lhsT=lhsT, rhs=rhs, start=start, stop=stop), R, W)

    def tr(self, out, in_, ident, R, W):
        self.op("pe", lambda e: e.transpose(out=out, in_=in_, identity=ident), R, W)

    def act(self, out, in_, func, R, W, bias=None, scale=None, accum_out=None):
        kw = {}
        if bias is not None:
            kw["bias"] = bias
        if scale is not None:
            kw["scale"] = scale
        if accum_out is not None:
            kw["accum_out"] = accum_out
        self.op("act", lambda e: e.activation(out=out, in_=in_, func=func, **kw), R, W)

    def tt(self, eng, out, in0, in1, op, R, W):
        self.op(eng, lambda e: e.tensor_tensor(out=out, in0=in0, in1=in1, op=op), R, W)

    def ts(self, eng, out, in0, s1, s2, op0, op1, R, W):
        if op1 is None:
            self.op(eng, lambda e: e.tensor_scalar(out=out, in0=in0, scalar1=s1, scalar2=None, op0=op0), R, W)
        else:
            self.op(eng, lambda e: e.tensor_scalar(out=out, in0=in0, scalar1=s1, scalar2=s2, op0=op0, op1=op1), R, W)

    def stt(self, out, in0, scalar, in1, op0, op1, R, W):
        self.op("dve", lambda e: e.scalar_tensor_tensor(out=out, in0=in0, scalar=scalar, in1=in1, op0=op0, op1=op1), R, W)

    def cp(self, eng, out, in_, R, W):
        if eng == "act":
            self.op("act", lambda e: e.activation(out=out, in_=in_, func=AF.Copy), R, W)
        else:
            self.op(eng, lambda e: e.tensor_copy(out=out, in_=in_), R, W)

    def red(self, out, in_, op, R, W):
        self.op("dve", lambda e: e.tensor_reduce(out=out, in_=in_, axis=AX.X, op=op), R, W)

    def recip(self, out, in_, R, W):
        self.op("dve", lambda e: e.reciprocal(out=out, in_=in_), R, W)

    def ms(self, eng, ap, val, W):
        self.op(eng, lambda e: e.memset(ap, val), (), W)


def bcl(ap, n):
    sh = list(ap.shape)
    return ap.unsqueeze(len(sh)).to_broadcast(sh + [n])


def build(NQ, NKP, NKS, phases=None):
    import os
    if phases is None:
        phases = os.environ.get('K_PHASES', 'PH0,P0,PH1,PH2,PH3,PH4,PH5').split(',')
    nc = bass.Bass("TRN2", target_bir_lowering=False)
    fw = FW(nc)
    NKs = [NKP, NKS]

    def din(name, shape, dt=F32):
        return nc.dram_tensor(name, list(shape), dt, kind="ExternalInput").ap()

    def dscr(name, shape, dt):
        return nc.dram_tensor(name, list(shape), dt, kind="Internal").ap()

    xk = [din("xk_p", [NKP, D]), din("xk_s", [NKS, D])]
    xq = [din("xq_p", [NQ + 2, D]), din("xq_s", [NQ + 2, D])]
    hm_d = din("hmask", [128, 2, 2])
    cT_d = din("cT2", [128, DC, 2])
    cosk = [din("cosk_p", [NKP, 16]), din("cosk_s", [NKS, 16])]
    sink = [din("sink_p", [NKP, 16]), din("sink_s", [NKS, 16])]
    cosq = [din("cosq_p", [NQ, 16]), din("cosq_s", [NQ, 16])]
    sinq = [din("sinq_p", [NQ, 16]), din("sinq_s", [NQ, 16])]
    w_ada_d = din("w_ada", [D, 6 * D])
    b_ada_d = din("b_ada2", [2, 6 * D])
    g1T_d = din("g1T", [128, DC])
    g2T_d = din("g2T", [128, DC])
    w_in_d = din("w_in", [D, 4000])
    convw_d = din("convw", [128, 4, 3])
    wco_d = din("w_conv_out", [512, D])
    gq_d = din("gq_bc", [128, 256])
    wuq_d = din("w_uq", [256, 768])
    gkv_d = din("gkv_bc", [128, 128])
    wukv_d = din("w_ukv", [128, D])
    gqn_d = din("gqn_bc", [128, 96])
    gkn_d = din("gkn_bc", [128, 96])
    wao_d = din("w_attn_out", [512, D])
    wout_d = din("w_out", [D, D])
    wq_d = din("peer_wq", [D, D])
    k12_d = din("k12T", [128, 8, 128])
    if TINY_TABLES:
        ut_d = din("UT", [1, 128, D])
        v_d = din("peer_v", [128, D])
    else:
        ut_d = din("UT", [NCH, 128, D])
        v_d = din("peer_v", [NE, D])
    ident_d = din("ident", [128, 128])
    iota_d = din("iota", [128, 128])
    selA_d = din("selA", [2, 2, 128])
    sel65_d = din("sel65", [65, 64])
    out_d = [nc.dram_tensor("out_p", [NQ, D], F32, kind="ExternalOutput").ap(),
             nc.dram_tensor("out_s", [NQ, D], F32, kind="ExternalOutput").ap()]

    KT = [dscr("KT_p", [8, 96, NKP], BF16), dscr("KT_s", [8, 96, NKS], BF16)]
    VP = [dscr("VP_p", [8, 128, NKP // 128, 66], BF16), dscr("VP_s", [8, 128, NKS // 128, 66], BF16)]
    QT = [dscr("QT_p", [8, 96, NQ], BF16), dscr("QT_s", [8, 96, NQ], BF16)]
    MA = [dscr("MA_p", [8, 128, NQ], BF16), dscr("MA_s", [8, 128, NQ], BF16)]
    SGB = [dscr("SGB_p", [8, 128, NQ], BF16), dscr("SGB_s", [8, 128, NQ], BF16)]
    OB = [dscr("OB_p", [8, 64, NQ], BF16), dscr("OB_s", [8, 64, NQ], BF16)]
    Y1 = [dscr("Y1_p", [NQ, D], F32), dscr("Y1_s", [NQ, D], F32)]
    GATE = dscr("GATE", [2, 2, 128, D], F32)
    UTb = dscr("UTb", [NCH, 128, D], BF16)
    Vb = dscr("Vb", [NE, D], BF16)

    uid = [0]

    with contextlib.ExitStack() as top:
        def sb(es, name, shape, dt):
            uid[0] += 1
            nm = "%s_%d" % (name, uid[0])
            t = es.enter_context(nc.sbuf_tensor(nm, list(shape), dt))
            return T(t, fw.buf(nm), nm)

        def ring(es, name, shape, dt, n):
            return Ring([sb(es, "%s%d" % (name, i), shape, dt) for i in range(n)])

        psum = top.enter_context(nc.psum_tensor("psum", [128, 8, 512], F32))
        pbank = [fw.buf("bank%d" % i, True) for i in range(8)]

        def bank_f(i):
            return psum[:, i, :]

        def bank_b(i):
            return psum[:, i, :].bitcast(BF16)

        ident_f = sb(top, "identf", [128, 128], F32)
        ident_b = sb(top, "identb", [128, 128], BF16)
        iota_f = sb(top, "iota", [128, 128], F32)
        selA = sb(top, "selA", [2, 2, 128], F32)
        sel65 = sb(top, "sel65", [65, 64], F32)
        hm = sb(top, "hm", [128, 2, 2], F32)
        gq_bc = sb(top, "gq", [128, 256], F32)
        gkv_bc = sb(top, "gkv", [128, 128], F32)
        gqs_bc = sb(top, "gqs", [128, 96], F32)
        gkn_bc = sb(top, "gkn", [128, 96], F32)
        convw = sb(top, "convw", [128, 4, 3], F32)
        negB = sb(top, "negB", [128, 1], F32)
        gmod1 = [sb(top, "gmod1_%d" % r, [128, DC], F32) for r in range(2)]
        shift1 = [sb(top, "shift1_%d" % r, [128, DC], F32) for r in range(2)]
        gmod2 = [sb(top, "gmod2_%d" % r, [128, DC], F32) for r in range(2)]
        shift2 = [sb(top, "shift2_%d" % r, [128, DC], F32) for r in range(2)]
        wkv_b = sb(top, "wkv_b", [128, DC, 160], BF16)
        wukv_b = sb(top, "wukv_b", [128, D], BF16)

        def load(dst, src_ap, q="sp"):
            fw.dma(q, dst.t[:], src_ap, W=[dst.b], key=dst.name)

        with contextlib.ExitStack() as ph:
            load(ident_f, ident_d)
            load(iota_f, iota_d)
            load(selA, selA_d)
            load(sel65, sel65_d)
            load(hm, hm_d)
            load(gq_bc, gq_d)
            load(gkv_bc, gkv_d)
            load(gkn_bc, gkn_d)
            load(convw, convw_d)
            gqn_bc = sb(ph, "gqn", [128, 96], F32)
            load(gqn_bc, gqn_d)
            g1T = sb(ph, "g1T", [128, DC], F32)
            g2T = sb(ph, "g2T", [128, DC], F32)
            load(g1T, g1T_d)
            load(g2T, g2T_d)
            cT = sb(ph, "cT", [128, DC, 2], F32)
            load(cT, cT_d)
            bada = sb(ph, "bada", [2, 6 * D], F32)
            load(bada, b_ada_d)
            fw.cp("dve", ident_b.t[:], ident_f.t[:], [ident_f.b], [ident_b.b])
            fw.ts("dve", gqs_bc.t[:], gqn_bc.t[:], 96.0 ** -0.5, None, ALU.mult, None, [gqn_bc.b], [gqs_bc.b])
            sq_t = sb(ph, "sqt", [128, 96], F32)
            m1 = sb(ph, "m1", [128, 1], F32)
            m2 = sb(ph, "m2", [128, 1], F32)
            fw.tt("dve", sq_t.t[:], gqs_bc.t[:], gqs_bc.t[:], ALU.mult, [gqs_bc.b], [sq_t.b])
            fw.red(m1.t[:], sq_t.t[:], ALU.max, [sq_t.b], [m1.b])
            fw.tt("dve", sq_t.t[:], gkn_bc.t[:], gkn_bc.t[:], ALU.mult, [gkn_bc.b, m1.b], [sq_t.b])
            fw.red(m2.t[:], sq_t.t[:], ALU.max, [sq_t.b], [m2.b])
            fw.tt("dve", m1.t[:], m1.t[:], m2.t[:], ALU.mult, [m1.b, m2.b], [m1.b])
            fw.ts("dve", m1.t[:], m1.t[:], 96.0 * 96.0, None, ALU.mult, None, [m1.b], [m1.b])
            fw.act(m1.t[:], m1.t[:], AF.Sqrt, [m1.b], [m1.b])
            fw.ts("dve", negB.t[:], m1.t[:], -1.0, None, ALU.mult, None, [m1.b], [negB.b])

            stg_kv = sb(ph, "stgkv", [128, DC, 160], F32)
            fw.dma("sp", stg_kv.t[:], w_in_d[:, O_CKV:O_CKV + 160].rearrange("(c p) n -> p c n", p=128),
                   W=[stg_kv.b], key=stg_kv.name)
            fw.cp("dve", wkv_b.t[:], stg_kv.t[:], [stg_kv.b], [wkv_b.b])
            stg_ukv = sb(ph, "stgukv", [128, D], F32)
            load(stg_ukv, wukv_d)
            fw.cp("dve", wukv_b.t[:], stg_ukv.t[:], [stg_ukv.b], [wukv_b.b])

            ada = sb(ph, "ada", [2, 6 * D], F32)
            stg = ring(ph, "stgada", [128, DC, 512], F32, 2)
            for g in range(12):
                st = stg.get()
                fw.dma("sp", st.t[:], w_ada_d[:, g * 512:(g + 1) * 512].rearrange("(c p) n -> p c n", p=128),
                       W=[st.b], key=st.name)
                bk = g % 2
                for c in range(DC):
                    fw.mm(psum[0:2, bk, :], cT.t[:, c, :], st.t[:, c, :], c == 0, c == DC - 1,
                          [cT.b, st.b], [pbank[bk]])
                fw.tt("dve", ada.t[:, g * 512:(g + 1) * 512], psum[0:2, bk, :], bada.t[:, g * 512:(g + 1) * 512],
                      ALU.add, [pbank[bk], bada.b], [ada.b])
            adaT = sb(ph, "adaT", [128, 48, 2], F32)
            gst = ring(ph, "gst", [128, D], F32, 2)
            pv = psum[:, 2, 0:96].rearrange("p (n r) -> p n r", r=2)
            for n in range(48):
                fw.tr(pv[:, n, :], ada.t[0:2, n * 128:(n + 1) * 128], ident_f.t[0:2, 0:2],
                      [ada.b, ident_f.b], [pbank[2]])
            fw.cp("dve", adaT.t[:], pv, [pbank[2]], [adaT.b])
            for r in range(2):
                fw.stt(gmod1[r].t[:], adaT.t[:, 8:16, r], 1.0, g1T.t[:], ALU.add, ALU.mult,
                       [adaT.b, g1T.b], [gmod1[r].b])
                fw.cp("dve", shift1[r].t[:], adaT.t[:, 0:8, r], [adaT.b], [shift1[r].b])
                fw.stt(gmod2[r].t[:], adaT.t[:, 32:40, r], 1.0, g2T.t[:], ALU.add, ALU.mult,
                       [adaT.b, g2T.b], [gmod2[r].b])
                fw.cp("dve", shift2[r].t[:], adaT.t[:, 24:32, r], [adaT.b], [shift2[r].b])
                for (wh, off) in ((0, 2 * D), (1, 5 * D)):
                    dst = gst.get()
                    for half in range(2):
                        bk = 4 + half
                        fw.mm(psum[:, bk, :], selA.t[:, r, :], ada.t[:, off + half * 512: off + (half + 1) * 512],
                              True, True, [selA.b, ada.b], [pbank[bk]])
                        fw.cp("act", dst.t[:, half * 512:(half + 1) * 512], psum[:, bk, :], [pbank[bk]], [dst.b])
                    fw.dma("act", GATE[r, wh], dst.t[:], R=[dst.b], key=dst.name)
            fw.barrier()
            ph.close()

        def make_fe(es, n=2):
            S = {}
            S["junk"] = sb(es, "fejunk", [128, D], F32)
            S["ssq"] = ring(es, "fessq", [128, 1], F32, max(2, n))
            S["rstd"] = ring(es, "ferstd", [128, 1], F32, max(2, n))
            S["xs"] = ring(es, "fexs", [128, D], BF16, n)
            S["tmp"] = ring(es, "fetmp", [128, DC, 128], F32, n)
            return S

        def fe_gen(S, xt, rows, gm, sh, hT_ap, hT_buf, bk):
            ssq = S["ssq"].get()
            rstd = S["rstd"].get()
            xs = S["xs"].get()
            tmp = S["tmp"].get()
            junk = S["junk"]
            fw.act(junk.t[:rows], xt.t[:rows], AF.Square, [xt.b], [junk.b, ssq.b], accum_out=ssq.t[:rows])
            yield
            fw.ts("dve", rstd.t[:rows], ssq.t[:rows], 1.0 / D, EPS, ALU.mult, ALU.add, [ssq.b], [rstd.b])
            yield
            fw.act(rstd.t[:rows], rstd.t[:rows], AF.Sqrt, [rstd.b], [rstd.b])
            yield
            fw.recip(rstd.t[:rows], rstd.t[:rows], [rstd.b], [rstd.b])
            yield
            fw.act(xs.t[:rows], xt.t[:rows], AF.Copy, [xt.b, rstd.b], [xs.b], scale=rstd.t[:rows])
            yield
            pv = bank_b(bk).rearrange("p (c t) -> p c t", t=128)
            for c in range(DC):
                fw.tr(pv[:, c, :rows], xs.t[:rows, c * 128:(c + 1) * 128], ident_b.t[:rows, :rows],
                      [xs.b, ident_b.b], [pbank[bk]])
            yield
            fw.tt("dve", tmp.t[:, :, :rows], pv[:, :, :rows], bcl(gm.t[:], rows), ALU.mult,
                  [pbank[bk], gm.b], [tmp.b])
            yield
            fw.tt("pool", hT_ap, tmp.t[:, :, :rows], bcl(sh.t[:], rows), ALU.add, [tmp.b, sh.b], [hT_buf])
            yield

        def fe(S, xt, rows, gm, sh, hT_ap, hT_buf, bk):
            for _ in fe_gen(S, xt, rows, gm, sh, hT_ap, hT_buf, bk):
                pass

        def rms_small_gen(S, src_ap, src_bufs, n, rows=128):
            ssq = S["ssq"].get()
            rstd = S["rstd"].get()
            junk = S["junk"]
            fw.act(junk.t[:rows, 0:n], src_ap, AF.Square, src_bufs, [junk.b, ssq.b], accum_out=ssq.t[:rows])
            yield
            fw.ts("dve", rstd.t[:rows], ssq.t[:rows], 1.0 / n, EPS, ALU.mult, ALU.add, [ssq.b], [rstd.b])
            yield
            fw.act(rstd.t[:rows], rstd.t[:rows], AF.Sqrt, [rstd.b], [rstd.b])
            yield
            fw.recip(rstd.t[:rows], rstd.t[:rows], [rstd.b], [rstd.b])
            yield
            return rstd

        def rms_small(S, src_ap, src_bufs, n, rows=128):
            g = rms_small_gen(S, src_ap, src_bufs, n, rows)
            while True:
                try:
                    next(g)
                except StopIteration as e:
                    return e.value

        def make_qkn(es, n=2):
            S = {}
            S["junk"] = ring(es, "qkjunk", [128, 8, 96], F32, n)
            S["ssq"] = ring(es, "qkssq", [128, 8], F32, n)
            S["xn"] = ring(es, "qkxn", [128, 8, 96], F32, n)
            S["ra"] = ring(es, "qkra", [128, 8, 16], F32, 2 * n)
            S["rb"] = ring(es, "qkrb", [128, 8, 16], F32, 2 * n)
            S["cs"] = ring(es, "qkcs", [128, 2, 16], F32, n + 1)
            return S

        def qknorm_rope_gen(S, xf, g_bc, cos_d, sin_d, out_ap, out_buf):
            cs = S["cs"].get()
            fw.dma("sp", cs.t[:, 0, :], cos_d, W=[cs.b], key=cs.name)
            yield
            fw.dma("sp", cs.t[:, 1, :], sin_d, W=[cs.b], key=cs.name)
            yield
            junk = S["junk"].get()
            ssq = S["ssq"].get()
            xn = S["xn"].get()
            fw.act(junk.t[:], xf.t[:], AF.Square, [xf.b], [junk.b])
            yield
            fw.red(ssq.t[:], junk.t[:], ALU.add, [junk.b], [ssq.b])
            yield
            fw.ts("dve", ssq.t[:], ssq.t[:], 1.0 / 96, EPS, ALU.mult, ALU.add, [ssq.b], [ssq.b])
            yield
            fw.act(ssq.t[:], ssq.t[:], AF.Sqrt, [ssq.b], [ssq.b])
            yield
            fw.recip(ssq.t[:], ssq.t[:], [ssq.b], [ssq.b])
            yield
            fw.tt("dve", xn.t[:], xf.t[:], bcl(ssq.t[:], 96), ALU.mult, [xf.b, ssq.b], [xn.b])
            yield
            fw.tt("dve", xn.t[:], xn.t[:], g_bc.t[:, None, :].to_broadcast([128, 8, 96]), ALU.mult,
                  [xn.b, g_bc.b], [xn.b])
            yield
            fw.cp("act", out_ap[:, :, 0:64], xn.t[:, :, 0:64], [xn.b], [out_buf])
            yield
            ra = S["ra"].get()
            rb = S["rb"].get()
            cb = cs.t[:, 0:1, :].to_broadcast([128, 8, 16])
            sbb = cs.t[:, 1:2, :].to_broadcast([128, 8, 16])
            t1 = xn.t[:, :, 64:80]
            t2 = xn.t[:, :, 80:96]
            fw.tt("pool", ra.t[:], t1, cb, ALU.mult, [xn.b, cs.b], [ra.b])
            yield
            fw.tt("pool", rb.t[:], t2, sbb, ALU.mult, [xn.b, cs.b], [rb.b])
            yield
            fw.tt("pool", out_ap[:, :, 64:80], ra.t[:], rb.t[:], ALU.subtract, [ra.b, rb.b], [out_buf])
            yield
            ra = S["ra"].get()
            rb = S["rb"].get()
            fw.tt("pool", ra.t[:], t1, sbb, ALU.mult, [xn.b, cs.b], [ra.b])
            yield
            fw.tt("pool", rb.t[:], t2, cb, ALU.mult, [xn.b, cs.b], [rb.b])
            yield
            fw.tt("pool", out_ap[:, :, 80:96], ra.t[:], rb.t[:], ALU.add, [ra.b, rb.b], [out_buf])
            yield

        def qknorm_rope(S, xf, g_bc, cos_d, sin_d, out_ap, out_buf):
            for _ in qknorm_rope_gen(S, xf, g_bc, cos_d, sin_d, out_ap, out_buf):
                pass

        for ph in ([contextlib.ExitStack()] if 'PH1' in phases else []):
            FE = make_fe(ph, 4)
            QK = make_qkn(ph, 2)
            xr = ring(ph, "k_x", [128, D], F32, 3)
            hTr = ring(ph, "k_hT", [128, DC, 128], BF16, 2)
            cn_r = ring(ph, "k_cn", [128, 128], BF16, 2)
            cTr = ring(ph, "k_cT", [128, 128], BF16, 2)
            kf_r = ring(ph, "k_kf", [128, 8, 96], F32, 2)
            kb_r = ring(ph, "k_kb", [128, 8, 96], BF16, 2)
            ktb_r = ring(ph, "k_ktb", [96, 8, 512], BF16, 2)
            vpb_r = ring(ph, "k_vpb", [128, 8, 4, 66], BF16, 2)
            for t_ in vpb_r.tiles:
                fw.ms("pool", t_.t[:], 1.0, [t_.b])

            su = ring(ph, "p0su", [128, D], F32, 2)
            sv = ring(ph, "p0sv", [128, D], F32, 2)
            bu = ring(ph, "p0bu", [128, D], BF16, 2)
            bv = ring(ph, "p0bv", [128, D], BF16, 2)

            def p0_gen():
                for c in range(NCH if 'P0' in phases else 0):
                    a_ = su.get()
                    b_ = bu.get()
                    fw.dma("sp", a_.t[:], ut_d[c], W=[a_.b], key=a_.name)
                    fw.cp("act", b_.t[:], a_.t[:], [a_.b], [b_.b])
                    fw.dma("act", UTb[c], b_.t[:], R=[b_.b], key=b_.name)
                    a_ = sv.get()
                    b_ = bv.get()
                    fw.dma("sp", a_.t[:], v_d[c * 128:(c + 1) * 128, :], W=[a_.b], key=a_.name)
                    fw.cp("dve", b_.t[:], a_.t[:], [a_.b], [b_.b])
                    fw.dma("act", Vb[c * 128:(c + 1) * 128, :], b_.t[:], R=[b_.b], key=b_.name)
                    yield
            p0g = p0_gen()

            def k_tile(r, kb, i, ktb, vpb, par):
                X = 0 if par == 0 else 3
                K0 = 1 if par == 0 else 4
                t0 = (kb * 4 + i) * 128
                xt = xr.get()
                fw.dma("sp", xt.t[:], xk[r][t0:t0 + 128, :], W=[xt.b], key=xt.name)
                hT = hTr.get()
                yield from fe_gen(FE, xt, 128, gmod1[r], shift1[r], hT.t[:], hT.b, X)
                pk = psum[:, X, 0:160]
                for c in range(DC):
                    fw.mm(pk, hT.t[:, c, :], wkv_b.t[:, c, :], c == 0, c == DC - 1, [hT.b, wkv_b.b], [pbank[X]])
                yield
                rstd = yield from rms_small_gen(FE, psum[:, X, 0:128], [pbank[X]], 128)
                cn = cn_r.get()
                fw.stt(cn.t[:], psum[:, X, 0:128], rstd.t[:], gkv_bc.t[:], ALU.mult, ALU.mult,
                       [pbank[X], rstd.b, gkv_bc.b], [cn.b])
                kf = kf_r.get()
                fw.cp("dve", kf.t[:, :, 64:96], psum[:, X, 128:160][:, None, :].to_broadcast([128, 8, 32]),
                      [pbank[X]], [kf.b])
                yield
                pT = bank_b(X)[:, 0:128]
                fw.tr(pT, cn.t[:], ident_b.t[:], [cn.b, ident_b.b], [pbank[X]])
                yield
                cTt = cTr.get()
                fw.cp("dve", cTt.t[:], pT, [pbank[X]], [cTt.b])
                yield
                for half in range(2):
                    fw.mm(psum[:, K0 + half, :], cTt.t[:], wukv_b.t[:, half * 512:(half + 1) * 512], True, True,
                          [cTt.b, wukv_b.b], [pbank[K0 + half]])
                yield
                for half in range(2):
                    kvv = psum[:, K0 + half, :].rearrange("p (h d) -> p h d", d=128)
                    hs = slice(half * 4, half * 4 + 4)
                    fw.cp("act", kf.t[:, hs, 0:64], kvv[:, :, 0:64], [pbank[K0 + half]], [kf.b])
                    fw.cp("dve", vpb.t[:, hs, i, 0:64], kvv[:, :, 64:128], [pbank[K0 + half]], [vpb.b])
                yield
                kbt = kb_r.get()
                yield from qknorm_rope_gen(QK, kf, gkn_bc, cosk[r][t0:t0 + 128, :], sink[r][t0:t0 + 128, :], kbt.t, kbt.b)
                pkt = bank_b(X).rearrange("p (h t) -> p h t", t=128)
                for h in range(8):
                    fw.tr(pkt[0:96, h, :], kbt.t[:, h, :], ident_b.t[:], [kbt.b, ident_b.b], [pbank[X]])
                yield
                fw.cp("act", ktb.t[:, :, i * 128:(i + 1) * 128], pkt[0:96, :, :], [pbank[X]], [ktb.b])
                yield

            for r in range(2):
                NK = NKs[r]
                for kb in range(NK // 512):
                    ktb = ktb_r.get()
                    vpb = vpb_r.get()
                    for pair in range(2):
                        gens = [k_tile(r, kb, pair * 2 + par, ktb, vpb, par) for par in range(2)]
                        while gens:
                            for g in list(gens):
                                if next(g, "done") == "done":
                                    gens.remove(g)
                        next(p0g, None)
                        next(p0g, None)
                    fw.dma("act", KT[r][:, :, kb * 512:(kb + 1) * 512].rearrange("h d k -> d h k"), ktb.t[:],
                           R=[ktb.b], key=ktb.name)
                    fw.dma("act", VP[r][:, :, kb * 4:(kb + 1) * 4, :].rearrange("h p k v -> p h k v"), vpb.t[:],
                           R=[vpb.b], key=vpb.name)
            for _ in p0g:
                pass
            fw.barrier()
            ph.close()

        for ph in ([contextlib.ExitStack()] if 'PH2' in phases else []):
            FE = make_fe(ph)
            QK = make_qkn(ph)
            w_in_b = sb(ph, "w_in_b", [128, DC, 4000], BF16)
            wco_b = sb(ph, "wco_b", [128, 4, D], BF16)
            wuq_b = sb(ph, "wuq_b", [128, 2, 768], BF16)
            with contextlib.ExitStack() as ld:
                stg = ring(ld, "stgin", [128, DC, 500], F32, 2)
                for g in range(8):
                    st = stg.get()
                    fw.dma("sp", st.t[:], w_in_d[:, g * 500:(g + 1) * 500].rearrange("(c p) n -> p c n", p=128),
                           W=[st.b], key=st.name)
                    fw.cp("dve" if g % 2 == 0 else "pool", w_in_b.t[:, :, g * 500:(g + 1) * 500], st.t[:],
                          [st.b], [w_in_b.b])
                st2 = sb(ld, "stgco", [128, 4, D], F32)
                fw.dma("sp", st2.t[:], wco_d.rearrange("(c p) n -> p c n", p=128), W=[st2.b], key=st2.name)
                fw.cp("dve", wco_b.t[:], st2.t[:], [st2.b], [wco_b.b])
                st3 = sb(ld, "stguq", [128, 2, 768], F32)
                fw.dma("sp", st3.t[:], wuq_d.rearrange("(c p) n -> p c n", p=128), W=[st3.b], key=st3.name)
                fw.cp("dve", wuq_b.t[:], st3.t[:], [st3.b], [wuq_b.b])
                fw.barrier()
            xr = ring(ph, "o_x", [128, D], F32, 2)
            hTb_r = ring(ph, "o_hTb", [128, DC, 512], BF16, 1)
            hTh_r = ring(ph, "o_hTh", [128, DC, 2], BF16, 2)
            zext_r = ring(ph, "o_zext", [128, 4, 514], F32, 1)
            cg_r = ring(ph, "o_cg", [128, 512], F32, 2)
            cgh = sb(ph, "o_cgh", [128, 4, 2], F32)
            zh = sb(ph, "o_zh", [128, 4, 2], F32)
            y_r = ring(ph, "o_y", [128, 512], F32, 2)
            uT_r = ring(ph, "o_uT", [128, 4, 512], BF16, 2)
            sg_r = ring(ph, "o_sg", [128, 512], F32, 2)
            maT_r = ring(ph, "o_maT", [128, 8, 512], BF16, 1)
            sgbT_r = ring(ph, "o_sgbT", [128, 8, 512], BF16, 1)
            cqn_r = ring(ph, "o_cqn", [128, 256], BF16, 2)
            cqT_r = ring(ph, "o_cqT", [128, 2, 128], BF16, 2)
            qf_r = ring(ph, "o_qf", [128, 8, 96], F32, 2)
            qb_r = ring(ph, "o_qb", [128, 8, 96], BF16, 2)
            qTb_r = ring(ph, "o_qTb", [96, 8, 512], BF16, 1)
            for r in range(2):
                for blk in range(NQ // 512):
                    hTb = hTb_r.get()
                    hTh = hTh_r.get()
                    for i in range(4):
                        t0 = 1 + blk * 512 + i * 128
                        xt = xr.get()
                        fw.dma("sp", xt.t[:], xq[r][t0:t0 + 128, :], W=[xt.b], key=xt.name)
                        fe(FE, xt, 128, gmod1[r], shift1[r], hTb.t[:, :, i * 128:(i + 1) * 128], hTb.b, 0)
                    xt = xr.get()
                    fw.dma("sp", xt.t[0:1], xq[r][blk * 512:blk * 512 + 1, :], W=[xt.b], key=xt.name)
                    fw.dma("sp", xt.t[1:2], xq[r][blk * 512 + 513:blk * 512 + 514, :], W=[xt.b], key=xt.name)
                    fe(FE, xt, 2, gmod1[r], shift1[r], hTh.t[:], hTh.b, 0)
                    zext = zext_r.get()
                    for j in range(4):
                        for c in range(DC):
                            fw.mm(psum[:, 1, :], w_in_b.t[:, c, 512 + j * 128:512 + (j + 1) * 128], hTb.t[:, c, :],
                                  c == 0, c == DC - 1, [w_in_b.b, hTb.b], [pbank[1]])
                        cg = cg_r.get()
                        fw.cp("act", cg.t[:], psum[:, 1, :], [pbank[1]], [cg.b])
                        for c in range(DC):
                            fw.mm(psum[:, 2, :], w_in_b.t[:, c, 1024 + j * 128:1024 + (j + 1) * 128], hTb.t[:, c, :],
                                  c == 0, c == DC - 1, [w_in_b.b, hTb.b], [pbank[2]])
                        fw.tt("dve", zext.t[:, j, 1:513], psum[:, 2, :], cg.t[:], ALU.mult, [pbank[2], cg.b], [zext.b])
                    phv = psum[:, 3, 0:16].rearrange("p (j w c) -> p j w c", j=4, w=2)
                    for j in range(4):
                        for w_ in range(2):
                            base = 512 + w_ * 512 + j * 128
                            for c in range(DC):
                                fw.mm(phv[:, j, w_, :], w_in_b.t[:, c, base:base + 128], hTh.t[:, c, :],
                                      c == 0, c == DC - 1, [w_in_b.b, hTh.b], [pbank[3]])
                    fw.cp("act", cgh.t[:], phv[:, :, 0, :], [pbank[3]], [cgh.b])
                    fw.tt("dve", zh.t[:], phv[:, :, 1, :], cgh.t[:], ALU.mult, [pbank[3], cgh.b], [zh.b])
                    if blk == 0:
                        fw.ts("dve", zh.t[:, :, 0:1], zh.t[:, :, 0:1], hm.t[:, r, 0:1], None, ALU.mult, None,
                              [zh.b, hm.b], [zh.b])
                    if blk == NQ // 512 - 1:
                        fw.ts("dve", zh.t[:, :, 1:2], zh.t[:, :, 1:2], hm.t[:, r, 1:2], None, ALU.mult, None,
                              [zh.b, hm.b], [zh.b])
                    fw.cp("dve", zext.t[:, :, 0:1], zh.t[:, :, 0:1], [zh.b], [zext.b])
                    fw.cp("dve", zext.t[:, :, 513:514], zh.t[:, :, 1:2], [zh.b], [zext.b])
                    uT = uT_r.get()
                    for j in range(4):
                        y = y_r.get()
                        fw.ts("dve", y.t[:], zext.t[:, j, 0:512], convw.t[:, j, 0:1], None, ALU.mult, None,
                              [zext.b, convw.b], [y.b])
                        fw.stt(y.t[:], zext.t[:, j, 1:513], convw.t[:, j, 1:2], y.t[:], ALU.mult, ALU.add,
                               [zext.b, convw.b, y.b], [y.b])
                        fw.stt(y.t[:], zext.t[:, j, 2:514], convw.t[:, j, 2:3], y.t[:], ALU.mult, ALU.add,
                               [zext.b, convw.b, y.b], [y.b])
                        for c in range(DC):
                            fw.mm(psum[:, 1 + (j % 2), :], w_in_b.t[:, c, j * 128:(j + 1) * 128], hTb.t[:, c, :],
                                  c == 0, c == DC - 1, [w_in_b.b, hTb.b], [pbank[1 + (j % 2)]])
                        fw.tt("dve", uT.t[:, j, :], psum[:, 1 + (j % 2), :], y.t[:], ALU.mult,
                              [pbank[1 + (j % 2)], y.b], [uT.b])
                    maT = maT_r.get()
                    sgbT = sgbT_r.get()
                    for m in range(8):
                        for j in range(4):
                            fw.mm(psum[:, 4, :], wco_b.t[:, j, m * 128:(m + 1) * 128], uT.t[:, j, :], j == 0, j == 3,
                                  [wco_b.b, uT.b], [pbank[4]])
                        for c in range(DC):
                            fw.mm(psum[:, 5, :], w_in_b.t[:, c, O_GA + m * 128:O_GA + (m + 1) * 128], hTb.t[:, c, :],
                                  c == 0, c == DC - 1, [w_in_b.b, hTb.b], [pbank[5]])
                        sg = sg_r.get()
                        fw.act(sg.t[:], psum[:, 5, :], AF.Sigmoid, [pbank[5]], [sg.b])
                        fw.tt("dve", maT.t[:, m, :], psum[:, 4, :], sg.t[:], ALU.mult, [pbank[4], sg.b], [maT.b])
                        for c in range(DC):
                            fw.mm(psum[:, 6, :], w_in_b.t[:, c, O_GB + m * 128:O_GB + (m + 1) * 128], hTb.t[:, c, :],
                                  c == 0, c == DC - 1, [w_in_b.b, hTb.b], [pbank[6]])
                        fw.act(sgbT.t[:, m, :], psum[:, 6, :], AF.Sigmoid, [pbank[6]], [sgbT.b])
                    sl = slice(blk * 512, (blk + 1) * 512)
                    fw.dma("act", MA[r][:, :, sl].rearrange("m p t -> p m t"), maT.t[:], R=[maT.b], key=maT.name)
                    fw.dma("act", SGB[r][:, :, sl].rearrange("m p t -> p m t"), sgbT.t[:], R=[sgbT.b], key=sgbT.name)
                    qTb = qTb_r.get()
                    for i in range(4):
                        for c in range(DC):
                            fw.mm(psum[:, 7, 0:256], hTb.t[:, c, i * 128:(i + 1) * 128], w_in_b.t[:, c, O_CQ:O_CQ + 256],
                                  c == 0, c == DC - 1, [hTb.b, w_in_b.b], [pbank[7]])
                        rstd = rms_small(FE, psum[:, 7, 0:256], [pbank[7]], 256)
                        cqn = cqn_r.get()
                        fw.stt(cqn.t[:], psum[:, 7, 0:256], rstd.t[:], gq_bc.t[:], ALU.mult, ALU.mult,
                               [pbank[7], rstd.b, gq_bc.b], [cqn.b])
                        ptq = bank_b(3)[:, 0:256].rearrange("p (c t) -> p c t", t=128)
                        for c2 in range(2):
                            fw.tr(ptq[:, c2, :], cqn.t[:, c2 * 128:(c2 + 1) * 128], ident_b.t[:],
                                  [cqn.b, ident_b.b], [pbank[3]])
                        cqT = cqT_r.get()
                        fw.cp("dve", cqT.t[:], ptq, [pbank[3]], [cqT.b])
                        for c2 in range(2):
                            fw.mm(psum[:, 1, :], cqT.t[:, c2, :], wuq_b.t[:, c2, 0:512], c2 == 0, c2 == 1,
                                  [cqT.b, wuq_b.b], [pbank[1]])
                        for c2 in range(2):
                            fw.mm(psum[:, 2, 0:256], cqT.t[:, c2, :], wuq_b.t[:, c2, 512:768], c2 == 0, c2 == 1,
                                  [cqT.b, wuq_b.b], [pbank[2]])
                        qf = qf_r.get()
                        qfv = qf.t[:].rearrange("p h d -> p (h d)")
                        fw.cp("act", qfv[:, 0:512], psum[:, 1, :], [pbank[1]], [qf.b])
                        fw.cp("act", qfv[:, 512:768], psum[:, 2, 0:256], [pbank[2]], [qf.b])
                        qb = qb_r.get()
                        q0 = blk * 512 + i * 128
                        qknorm_rope(QK, qf, gqs_bc, cosq[r][q0:q0 + 128, :], sinq[r][q0:q0 + 128, :], qb.t, qb.b)
                        pkt = bank_b(3).rearrange("p (h t) -> p h t", t=128)
                        for h in range(8):
                            fw.tr(pkt[0:96, h, :], qb.t[:, h, :], ident_b.t[:], [qb.b, ident_b.b], [pbank[3]])
                        fw.cp("act", qTb.t[:, :, i * 128:(i + 1) * 128], pkt[0:96, :, :], [pbank[3]], [qTb.b])
                    fw.dma("act", QT[r][:, :, sl].rearrange("h d k -> d h k"), qTb.t[:], R=[qTb.b], key=qTb.name)
            fw.barrier()
            ph.close()

        for ph in ([contextlib.ExitStack()] if 'PH3' in phases else []):
            NKmax = max(NKs)
            kt_r = ring(ph, "a_kt", [96, NKmax], BF16, 2)
            vp_r = ring(ph, "a_vp", [128, NKmax // 128, 66], BF16, 2)
            qt_r = ring(ph, "a_qt", [96, NQ], BF16, 2)
            pT_r = ring(ph, "a_pT", [128, 512], BF16, 4)
            osb_r = ring(ph, "a_osb", [65, 512], F32, 2)
            rl_r = ring(ph, "a_rl", [64, 512], F32, 2)
            obh_r = ring(ph, "a_obh", [64, NQ], BF16, 2)
            ps_box = [0]
            po_i = 0
            for r in range(2):
                NK = NKs[r]
                nkt = NK // 128
                for h in range(8):
                    ktt = kt_r.get()
                    vpt = vp_r.get()
                    qtt = qt_r.get()
                    fw.dma("sp", ktt.t[:, 0:NK], KT[r][h], W=[ktt.b], key=ktt.name)
                    fw.dma("sp", vpt.t[:, 0:nkt, :], VP[r][h], W=[vpt.b], key=vpt.name)
                    fw.dma("sp", qtt.t[:], QT[r][h], W=[qtt.b], key=qtt.name)
                    obh = obh_r.get()
                    for qc in range(NQ // 512):
                        pob = 4 + (po_i % 2)
                        po_i += 1
                        LOOK = 2
                        sbanks = {}

                        def issue_S(j, qc=qc, ktt=ktt, qtt=qtt):
                            nonlocal_ps = ps_box[0] % 4
                            ps_box[0] += 1
                            sbanks[j] = nonlocal_ps
                            fw.mm(psum[:, nonlocal_ps, :], ktt.t[:, j * 128:(j + 1) * 128], qtt.t[:, qc * 512:(qc + 1) * 512],
                                  True, True, [ktt.b, qtt.b], [pbank[nonlocal_ps]])
                        for j in range(min(LOOK, nkt)):
                            issue_S(j)
                        for kt in range(nkt):
                            if kt + LOOK < nkt:
                                issue_S(kt + LOOK)
                            psb = sbanks.pop(kt)
                            pT = pT_r.get()
                            fw.act(pT.t[:], psum[:, psb, :], AF.Exp, [pbank[psb], negB.b], [pT.b], bias=negB.t[:])
                            fw.mm(psum[0:65, pob, :], vpt.t[:, kt, 0:65], pT.t[:], kt == 0, kt == nkt - 1,
                                  [vpt.b, pT.b], [pbank[pob]])
                        osb = osb_r.get()
                        fw.cp("dve", osb.t[:], psum[0:65, pob, :], [pbank[pob]], [osb.b])
                        fw.mm(psum[0:64, 6, :], sel65.t[:], osb.t[:], True, True, [sel65.b, osb.b], [pbank[6]])
                        rl = rl_r.get()
                        fw.recip(rl.t[:], psum[0:64, 6, :], [pbank[6]], [rl.b])
                        fw.tt("pool", obh.t[:, qc * 512:(qc + 1) * 512], osb.t[0:64, :], rl.t[:], ALU.mult,
                              [osb.b, rl.b], [obh.b])
                    fw.dma("act", OB[r][h], obh.t[:], R=[obh.b], key=obh.name)
            fw.barrier()
            ph.close()

        for ph in ([contextlib.ExitStack()] if 'PH4' in phases else []):
            wao_b = sb(ph, "wao_b", [64, 8, D], BF16)
            woutg = sb(ph, "woutg", [128, DC, D], BF16)
            with contextlib.ExitStack() as ld:
                st = sb(ld, "stgao", [64, 8, D], F32)
                fw.dma("sp", st.t[:], wao_d.rearrange("(h p) n -> p h n", p=64), W=[st.b], key=st.name)
                fw.cp("dve", wao_b.t[:], st.t[:], [st.b], [wao_b.b])
                fw.barrier()
            stw = sb(ph, "stgwo", [128, DC, D], F32)
            fw.dma("sp", stw.t[:], wout_d.rearrange("(c p) n -> p c n", p=128), W=[stw.b], key=stw.name)
            ma_r = ring(ph, "m_ma", [128, 8, 512], BF16, 2)
            sgb_r = ring(ph, "m_sgb", [128, 8, 512], BF16, 2)
            ob_r = ring(ph, "m_ob", [64, 8, 512], BF16, 2)
            t1_r = ring(ph, "m_t1", [128, 512], F32, 2)
            mg_r = ring(ph, "m_mg", [128, 8, 512], BF16, 2)
            xr = ring(ph, "m_x", [128, D], F32, 3)
            y1_r = ring(ph, "m_y1", [128, D], F32, 3)
            g1bc = sb(ph, "g1bc", [128, D], F32)
            for r in range(2):
                fw.dma("sp", g1bc.t[:], GATE[r, 0], W=[g1bc.b], key=g1bc.name)
                for c in range(DC):
                    fw.tt("dve" if c % 2 == 0 else "pool", woutg.t[:, c, :], stw.t[:, c, :], g1bc.t[:], ALU.mult,
                          [stw.b, g1bc.b], [woutg.b])
                for blk in range(NQ // 512):
                    sl = slice(blk * 512, (blk + 1) * 512)
                    ma = ma_r.get()
                    sgb = sgb_r.get()
                    ob = ob_r.get()
                    fw.dma("sp", ma.t[:], MA[r][:, :, sl].rearrange("m p t -> p m t"), W=[ma.b], key=ma.name)
                    fw.dma("sp", sgb.t[:], SGB[r][:, :, sl].rearrange("m p t -> p m t"), W=[sgb.b], key=sgb.name)
                    fw.dma("sp", ob.t[:], OB[r][:, :, sl].rearrange("h p t -> p h t"), W=[ob.b], key=ob.name)
                    mg = mg_r.get()
                    for m in range(8):
                        bk = m % 2
                        for h in range(8):
                            fw.mm(psum[:, bk, :], wao_b.t[:, h, m * 128:(m + 1) * 128], ob.t[:, h, :], h == 0, h == 7,
                                  [wao_b.b, ob.b], [pbank[bk]])
                        t1 = t1_r.get()
                        fw.tt("dve", t1.t[:], psum[:, bk, :], sgb.t[:, m, :], ALU.mult, [pbank[bk], sgb.b], [t1.b])
                        fw.tt("pool", mg.t[:, m, :], t1.t[:], ma.t[:, m, :], ALU.add, [t1.b, ma.b], [mg.b])
                    for i in range(4):
                        t0 = blk * 512 + i * 128
                        xt = xr.get()
                        fw.dma("sp", xt.t[:], xq[r][1 + t0:1 + t0 + 128, :], W=[xt.b], key=xt.name)
                        pb0 = 2 + 2 * (i % 2)
                        for half in range(2):
                            for k in range(DC):
                                fw.mm(psum[:, pb0 + half, :], mg.t[:, k, i * 128:(i + 1) * 128],
                                      woutg.t[:, k, half * 512:(half + 1) * 512], k == 0, k == DC - 1,
                                      [mg.b, woutg.b], [pbank[pb0 + half]])
                        y1 = y1_r.get()
                        for half in range(2):
                            hs = slice(half * 512, (half + 1) * 512)
                            fw.tt("dve", y1.t[:, hs], psum[:, pb0 + half, :], xt.t[:, hs], ALU.add,
                                  [pbank[pb0 + half], xt.b], [y1.b])
                        fw.dma("act", Y1[r][t0:t0 + 128, :], y1.t[:], R=[y1.b], key=y1.name)
            fw.barrier()
            ph.close()

        TB = 256
        for ph in ([contextlib.ExitStack()] if 'PH5' in phases else []):
            FE = make_fe(ph, 1)
            wq_b = sb(ph, "wq_b", [128, DC, D], BF16)
            k12_b = sb(ph, "k12_b", [128, 8, 128], BF16)
            with contextlib.ExitStack() as ld:
                st = sb(ld, "stgwq", [128, DC, D], F32)
                fw.dma("sp", st.t[:], wq_d.rearrange("(c p) n -> p c n", p=128), W=[st.b], key=st.name)
                fw.cp("dve", wq_b.t[:], st.t[:], [st.b], [wq_b.b])
                st2 = sb(ld, "stgk12", [128, 8, 128], F32)
                load(st2, k12_d)
                fw.cp("dve", k12_b.t[:], st2.t[:], [st2.b], [k12_b.b])
                fw.barrier()
            Wsb = sb(ph, "Wsb", [128, 128, TB], BF16)
            y1t_r = ring(ph, "e_y1", [128, D], F32, 4)
            h2T_r = ring(ph, "e_h2T", [128, DC, TB], BF16, 2)
            qT_r = ring(ph, "e_qT", [128, 8, TB], BF16, 1)
            s12 = sb(ph, "e_s12", [128, 8, 2, 128], F32)
            s12w = sb(ph, "e_s12w", [128, 8, 2, 128], F32)
            v12 = sb(ph, "e_v12", [128, 8, 2, 16], F32)
            i12 = sb(ph, "e_i12", [128, 8, 2, 16], U32)
            i12f = sb(ph, "e_i12f", [128, 8, 2, 16], F32)
            comb = sb(ph, "e_comb", [128, 8, 256], F32)
            combw = sb(ph, "e_combw", [128, 8, 256], F32)
            sc = sb(ph, "e_sc", [128, 8, 16], F32)
            pos = sb(ph, "e_pos", [128, 8, 16], U32)
            posa = sb(ph, "e_posa", [128, 8, 16], U32)
            posb = sb(ph, "e_posb", [128, 8, 16], U32)
            iaf = sb(ph, "e_iaf", [128, 8, 16], F32)
            ibf = sb(ph, "e_ibf", [128, 8, 16], F32)
            eq = sb(ph, "e_eq", [128, 4, 16, 16], F32)
            hlw = sb(ph, "e_hlw", [128, 3, 128], F32)
            ex = sb(ph, "e_ex", [128, 8, 16], F32)
            zz = sb(ph, "e_zz", [128, 8], F32)
            jT_r = ring(ph, "e_jT", [128, 3, TB], F32, 1)
            ohl_r = ring(ph, "e_ohl", [128, 8, 128], BF16, 2)
            ohh_r = ring(ph, "e_ohh", [128, 8, 128], BF16, 2)
            ut_r = ring(ph, "e_ut", [128, DC, 128], BF16, 2)
            vt_r = ring(ph, "e_vt", [128, D], BF16, 3)
            g1_r = ring(ph, "e_g1", [128, TB], BF16, 2)
            G_r = ring(ph, "e_G", [128, TB], BF16, 2)
            yo_r = ring(ph, "e_yo", [128, D], F32, 1)
            g2bc = sb(ph, "g2bc", [128, D], F32)
            hb = [(fw.buf("v12_%d" % i), fw.buf("i12_%d" % i), fw.buf("s12w_%d" % i)) for i in range(16)]
            hb2 = [(fw.buf("sc_%d" % i), fw.buf("pos_%d" % i), fw.buf("combw_%d" % i)) for i in range(8)]
            pw_box = [0]
            pa_box = [0]

            def prep_topk(r, blk, h2T, qT, jT, y1ts):
                for i in range(TB // 128):
                    t0 = blk * TB + i * 128
                    y1t = y1t_r.get()
                    fw.dma("sp", y1t.t[:], Y1[r][t0:t0 + 128, :], W=[y1t.b], key=y1t.name)
                    y1ts.append(y1t)
                    fe(FE, y1t, 128, gmod2[r], shift2[r], h2T.t[:, :, i * 128:(i + 1) * 128], h2T.b, 6)
                    yield
                for h in range(8):
                    bk = 6 + (h % 2)
                    for c in range(DC):
                        fw.mm(psum[:, bk, 0:TB], wq_b.t[:, c, h * 128:(h + 1) * 128], h2T.t[:, c, :],
                              c == 0, c == DC - 1, [wq_b.b, h2T.b], [pbank[bk]])
                    fw.cp("act", qT.t[:, h, :], psum[:, bk, 0:TB], [pbank[bk]], [qT.b])
                    if h % 2 == 1:
                        yield
                for i in range(TB // 128):
                    tsl = slice(i * 128, (i + 1) * 128)
                    pssb = [psum[:, 6 + a_, :].rearrange("p (h n) -> p h n", n=128) for a_ in range(2)]
                    for hg in range(2):
                        for hh in range(4):
                            h = hg * 4 + hh
                            fw.mm(pssb[0][:, hh, :], qT.t[0:64, h, tsl], k12_b.t[0:64, h, :], True, True,
                                  [qT.b, k12_b.b], [pbank[6]])
                            fw.mm(pssb[1][:, hh, :], qT.t[64:128, h, tsl], k12_b.t[64:128, h, :], True, True,
                                  [qT.b, k12_b.b], [pbank[7]])
                        for a_ in range(2):
                            fw.cp("act", s12.t[:, hg * 4:(hg + 1) * 4, a_, :], pssb[a_], [pbank[6 + a_]], [s12.b])
                        yield
                    for stage in range(5):
                        for ha in range(16):
                            h, a = ha // 2, ha % 2
                            bv, bi, bw = hb[ha]
                            if stage == 0:
                                fw.op("dve", lambda e, h=h, a=a: e.max(out=v12.t[:, h, a, 0:8], in_=s12.t[:, h, a, :]),
                                      [s12.b], [bv])
                            elif stage == 1:
                                fw.op("dve", lambda e, h=h, a=a: e.max_index(out=i12.t[:, h, a, 0:8], in_max=v12.t[:, h, a, 0:8],
                                                                             in_values=s12.t[:, h, a, :]), [s12.b, bv], [bi])
                            elif stage == 2:
                                fw.op("dve", lambda e, h=h, a=a: e.match_replace(out=s12w.t[:, h, a, :], in_to_replace=v12.t[:, h, a, 0:8],
                                                                                 in_values=s12.t[:, h, a, :], imm_value=NEG),
                                      [s12.b, bv], [bw])
                            elif stage == 3:
                                fw.op("dve", lambda e, h=h, a=a: e.max(out=v12.t[:, h, a, 8:16], in_=s12w.t[:, h, a, :]),
                                      [bw], [bv])
                            else:
                                fw.op("dve", lambda e, h=h, a=a: e.max_index(out=i12.t[:, h, a, 8:16], in_max=v12.t[:, h, a, 8:16],
                                                                             in_values=s12w.t[:, h, a, :]), [bw, bv], [bi])
                            if ha % 8 == 7:
                                yield
                    allv = [x[0] for x in hb]
                    alli = [x[1] for x in hb]
                    combv = comb.t[:].rearrange("p h (a b) -> p h a b", b=16)
                    fw.tt("dve", combv, bcl(v12.t[:, :, 0, :], 16),
                          v12.t[:, :, 1, :][:, :, None, :].to_broadcast([128, 8, 16, 16]), ALU.add,
                          allv, [comb.b])
                    yield
                    for stage in range(5):
                        for h in range(8):
                            bs, bp, bw = hb2[h]
                            if stage == 0:
                                fw.op("dve", lambda e, h=h: e.max(out=sc.t[:, h, 0:8], in_=comb.t[:, h, :]), [comb.b], [bs])
                            elif stage == 1:
                                fw.op("dve", lambda e, h=h: e.max_index(out=pos.t[:, h, 0:8], in_max=sc.t[:, h, 0:8],
                                                                        in_values=comb.t[:, h, :]), [comb.b, bs], [bp])
                            elif stage == 2:
                                fw.op("dve", lambda e, h=h: e.match_replace(out=combw.t[:, h, :], in_to_replace=sc.t[:, h, 0:8],
                                                                            in_values=comb.t[:, h, :], imm_value=NEG),
                                      [comb.b, bs], [bw])
                            elif stage == 3:
                                fw.op("dve", lambda e, h=h: e.max(out=sc.t[:, h, 8:16], in_=combw.t[:, h, :]), [bw], [bs])
                            else:
                                fw.op("dve", lambda e, h=h: e.max_index(out=pos.t[:, h, 8:16], in_max=sc.t[:, h, 8:16],
                                                                        in_values=combw.t[:, h, :]), [bw, bs], [bp])
                        yield
                    alls = [x[0] for x in hb2]
                    allp = [x[1] for x in hb2]
                    fw.op("dve", lambda e: e.tensor_single_scalar(out=posa.t[:], in_=pos.t[:], scalar=4,
                                                                  op=ALU.logical_shift_right), allp, [posa.b])
                    fw.op("dve", lambda e: e.tensor_single_scalar(out=posb.t[:], in_=pos.t[:], scalar=15,
                                                                  op=ALU.bitwise_and), allp, [posb.b])
                    fw.cp("dve", iaf.t[:], posa.t[:], [posa.b], [iaf.b])
                    fw.cp("dve", ibf.t[:], posb.t[:], [posb.b], [ibf.b])
                    fw.cp("dve", i12f.t[:], i12.t[:], alli, [i12f.b])
                    yield
                    io16 = iota_f.t[:, 0:16][:, None, None, :].to_broadcast([128, 4, 16, 16])
                    for which, sel_f in ((0, iaf), (1, ibf)):
                        for hq in range(2):
                            hs = slice(hq * 4, hq * 4 + 4)
                            fw.tt("dve", eq.t[:], io16, bcl(sel_f.t[:, hs, :], 16), ALU.is_equal, [iota_f.b, sel_f.b], [eq.b])
                            fw.tt("dve", eq.t[:], eq.t[:],
                                  i12f.t[:, hs, which, :][:, :, None, :].to_broadcast([128, 4, 16, 16]), ALU.mult,
                                  [eq.b, i12f.b], [eq.b])
                            fw.red(hlw.t[:, which, hq * 64:(hq + 1) * 64].rearrange("p (h k) -> p h k", k=16), eq.t[:], ALU.add,
                                   [eq.b], [hlw.b])
                            yield
                    fw.tt("dve", ex.t[:], sc.t[:], bcl(sc.t[:, :, 0], 16), ALU.subtract, alls, [ex.b])
                    fw.act(ex.t[:], ex.t[:], AF.Exp, [ex.b], [ex.b])
                    fw.red(zz.t[:], ex.t[:], ALU.add, [ex.b], [zz.b])
                    fw.recip(zz.t[:], zz.t[:], [zz.b], [zz.b])
                    fw.tt("dve", hlw.t[:, 2, :].rearrange("p (h k) -> p h k", k=16), ex.t[:], bcl(zz.t[:], 16),
                          ALU.mult, [ex.b, zz.b], [hlw.b])
                    pj = psum[:, 6, 0:384].rearrange("p (w t) -> p w t", t=128)
                    for w_ in range(3):
                        fw.tr(pj[:, w_, :], hlw.t[:, w_, :], ident_f.t[:], [hlw.b, ident_f.b], [pbank[6]])
                    fw.cp("act", jT.t[:, :, tsl], pj, [pbank[6]], [jT.b])
                    yield

            def w_build(jT):
                for sub in range(TB // 8):
                    ohl = ohl_r.get()
                    ohh = ohh_r.get()
                    for u in range(8):
                        t = sub * 8 + u
                        fw.ts("dve", ohl.t[:, u, :], iota_f.t[:], jT.t[:, 1, t:t + 1], None, ALU.is_equal, None,
                              [iota_f.b, jT.b], [ohl.b])
                        fw.ts("dve", ohh.t[:, u, :], iota_f.t[:], jT.t[:, 0, t:t + 1], jT.t[:, 2, t:t + 1],
                              ALU.is_equal, ALU.mult, [iota_f.b, jT.b], [ohh.b])
                    for g in range(2):
                        bk = 6 + (pw_box[0] % 2)
                        pw_box[0] += 1
                        pwv = psum[:, bk, :].rearrange("p (u c) -> p u c", c=128)
                        for u in range(4):
                            fw.mm(pwv[:, u, :], ohl.t[:, g * 4 + u, :], ohh.t[:, g * 4 + u, :], True, True,
                                  [ohl.b, ohh.b], [pbank[bk]])
                        t0 = sub * 8 + g * 4
                        fw.cp("act", Wsb.t[:, :, t0:t0 + 4].rearrange("p c t -> p t c"), pwv, [pbank[bk]], [Wsb.b])

            def chunk_loop(h2T):
                ainfo = {}

                def issue_A(c):
                    ut = ut_r.get()
                    vt = vt_r.get()
                    fw.dma("sp", ut.t[:], UTb[c].rearrange("p (dc e) -> p dc e", e=128), W=[ut.b], key=ut.name)
                    fw.dma("sp", vt.t[:], Vb[c * 128:(c + 1) * 128, :], W=[vt.b], key=vt.name)
                    bk = 4 + (pa_box[0] % 2)
                    pa_box[0] += 1
                    for dc in range(DC):
                        fw.mm(psum[:, bk, 0:TB], ut.t[:, dc, :], h2T.t[:, dc, :], dc == 0, dc == DC - 1,
                              [ut.b, h2T.b], [pbank[bk]])
                    ainfo[c] = (bk, vt)
                issue_A(0)
                for c in range(NCH):
                    if c + 1 < NCH:
                        issue_A(c + 1)
                    bk, vt = ainfo.pop(c)
                    g1 = g1_r.get()
                    fw.act(g1.t[:], psum[:, bk, 0:TB], AF.Gelu_apprx_tanh, [pbank[bk]], [g1.b])
                    G = G_r.get()
                    fw.tt("dve", G.t[:], g1.t[:], Wsb.t[:, c, :], ALU.mult, [g1.b, Wsb.b], [G.b])
                    for i in range(TB // 128):
                        for half in range(2):
                            ab = i * 2 + half
                            fw.mm(psum[:, ab, :], G.t[:, i * 128:(i + 1) * 128], vt.t[:, half * 512:(half + 1) * 512],
                                  c == 0, c == NCH - 1, [G.b, vt.b], [pbank[ab]])
                    yield

            def final(r, blk, y1ts):
                if blk == 0:
                    fw.dma("sp", g2bc.t[:], GATE[r, 1], W=[g2bc.b], key=g2bc.name)
                for i in range(TB // 128):
                    t0 = blk * TB + i * 128
                    yo = yo_r.get()
                    for half in range(2):
                        hs = slice(half * 512, (half + 1) * 512)
                        fw.tt("dve", yo.t[:, hs], psum[:, 2 * i + half, :], g2bc.t[:, hs],
                              ALU.mult, [pbank[2 * i + half], g2bc.b], [yo.b])
                    fw.tt("pool", yo.t[:], yo.t[:], y1ts[i].t[:], ALU.add, [yo.b, y1ts[i].b], [yo.b])
                    fw.dma("act", out_d[r][t0:t0 + 128, :], yo.t[:], R=[yo.b], key=yo.name)

            blocks = [(r, blk) for r in range(2) for blk in range(NQ // TB)]
            jT = jT_r.get()
            qT = qT_r.get()

            def new_state():
                return {"h2T": h2T_r.get(), "y1ts": []}
            cur = new_state()
            for _ in prep_topk(blocks[0][0], blocks[0][1], cur["h2T"], qT, jT, cur["y1ts"]):
                pass
            for n, (r, blk) in enumerate(blocks):
                w_build(jT)
                nxt = None
                gen = None
                if n + 1 < len(blocks):
                    nxt = new_state()
                    gen = prep_topk(blocks[n + 1][0], blocks[n + 1][1], nxt["h2T"], qT, jT, nxt["y1ts"])
                for _ in chunk_loop(cur["h2T"]):
                    if gen is not None:
                        if next(gen, "done") == "done":
                            gen = None
                if gen is not None:
                    for _ in gen:
                        pass
                final(r, blk, cur["y1ts"])
                cur = nxt
            fw.barrier()
            ph.close()
        fw.finalize()
    return nc, fw


def _rope_tables(pos):
    pos = pos.astype(np.float32)
    inv = (np.float32(10000.0) ** (-np.arange(0, 32, 2, dtype=np.float32) / np.float32(32))).astype(np.float32)
    ang = (pos[:, None] * inv[None, :]).astype(np.float32)
    return np.cos(ang).astype(np.float32), np.sin(ang).astype(np.float32)


def prep_inputs(inp, NQ):
    f = lambda a: np.ascontiguousarray(np.asarray(a, dtype=np.float32))
    xp = f(inp["x_prompt"])
    xs = f(inp["x_sample"])
    NKP = xp.shape[1]
    NKS = xs.shape[1]
    assert NKP == 4 * NQ and NKS == 2 * NQ and xp.shape[0] == 2 and xs.shape[0] == 4

    def colT(v, nch):
        return np.ascontiguousarray(v.reshape(nch, 128).T)

    def rep(v):
        return np.ascontiguousarray(np.broadcast_to(v[None, :], (128, v.shape[0])))

    k1 = f(inp["peer_k1"])[0]
    k2 = f(inp["peer_k2"])[0]
    k12T = np.concatenate([k1.transpose(2, 0, 1), k2.transpose(2, 0, 1)], axis=0)
    U = f(inp["peer_u"])[0]
    UT = np.ascontiguousarray(U.reshape(NCH, 128, DC, 128).transpose(0, 3, 2, 1)).reshape(NCH, 128, D)
    convw = f(inp["conv_w"])[0]
    convw_l = np.ascontiguousarray(convw.reshape(3, 4, 128).transpose(2, 1, 0))
    selA = np.zeros((2, 2, 128), np.float32)
    selA[0, 0, :] = 1.0
    selA[1, 1, :] = 1.0
    sel65 = np.zeros((65, 64), np.float32)
    sel65[64, :] = 1.0
    shared = {
        "w_ada": f(inp["w_ada"])[0],
        "b_ada2": np.ascontiguousarray(np.broadcast_to(f(inp["b_ada"])[0][None, :], (2, 6 * D))),
        "g1T": colT(f(inp["g_norm1"])[0], DC),
        "g2T": colT(f(inp["g_norm2"])[0], DC),
        "w_in": f(inp["w_in"])[0],
        "convw": convw_l,
        "w_conv_out": f(inp["w_conv_out"])[0],
        "gq_bc": rep(f(inp["g_q_lora"])[0]),
        "w_uq": f(inp["w_uq"])[0],
        "gkv_bc": rep(f(inp["g_kv_lora"])[0]),
        "w_ukv": f(inp["w_ukv"])[0],
        "gqn_bc": rep(f(inp["g_qnorm"])[0]),
        "gkn_bc": rep(f(inp["g_knorm"])[0]),
        "w_attn_out": f(inp["w_attn_out"])[0],
        "w_out": f(inp["w_out"])[0],
        "peer_wq": f(inp["peer_wq"])[0],
        "k12T": np.ascontiguousarray(k12T),
        "UT": UT[:1] if TINY_TABLES else UT,
        "peer_v": f(inp["peer_v"])[0][:128] if TINY_TABLES else f(inp["peer_v"])[0],
        "ident": np.eye(128, dtype=np.float32),
        "iota": np.ascontiguousarray(np.broadcast_to(np.arange(128, dtype=np.float32)[None, :], (128, 128))),
        "selA": selA,
        "sel65": sel65,
    }
    cp = f(inp["c_prompt"])
    cs = f(inp["c_sample"])
    ckp, skp = _rope_tables(np.arange(NKP))
    cks, sks = _rope_tables(np.arange(NKS))
    maps = []
    for i in range(8):
        pb, pq = i // 4, i % 4
        sb_, sh = i // 2, i % 2
        m = dict(shared)
        m["xk_p"] = xp[pb]
        m["xk_s"] = xs[sb_]
        hmask = np.zeros((128, 2, 2), np.float32)
        for r, (xfull, q0, NK) in enumerate(((xp[pb], pq * NQ, NKP), (xs[sb_], sh * NQ, NKS))):
            ext = np.zeros((NQ + 2, D), np.float32)
            ext[1:NQ + 1] = xfull[q0:q0 + NQ]
            if q0 > 0:
                ext[0] = xfull[q0 - 1]
                hmask[:, r, 0] = 1.0
            if q0 + NQ < NK:
                ext[NQ + 1] = xfull[q0 + NQ]
                hmask[:, r, 1] = 1.0
            m["xq_p" if r == 0 else "xq_s"] = ext
        m["hmask"] = hmask
        m["cT2"] = np.ascontiguousarray(np.stack([colT(cp[pb], DC), colT(cs[sb_], DC)], axis=-1))
        m["cosk_p"], m["sink_p"] = ckp, skp
        m["cosk_s"], m["sink_s"] = cks, sks
        m["cosq_p"] = np.ascontiguousarray(ckp[pq * NQ:(pq + 1) * NQ])
        m["sinq_p"] = np.ascontiguousarray(skp[pq * NQ:(pq + 1) * NQ])
        m["cosq_s"] = np.ascontiguousarray(cks[sh * NQ:(sh + 1) * NQ])
        m["sinq_s"] = np.ascontiguousarray(sks[sh * NQ:(sh + 1) * NQ])
        maps.append(m)
    return maps, NKP, NKS


def run(inputs, NQ):
    maps, NKP, NKS = prep_inputs(inputs, NQ)
    nc, fw = build(NQ, NKP, NKS)
    res = run_bass_kernel_spmd(nc, maps, core_ids=list(range(8)))
    yp = np.zeros((2, NKP, D), np.float32)
    ys = np.zeros((4, NKS, D), np.float32)
    for i in range(8):
        pb, pq = i // 4, i % 4
        sb_, sh = i // 2, i % 2
        yp[pb, pq * NQ:(pq + 1) * NQ] = res.results[i]["out_p"]
        ys[sb_, sh * NQ:(sh + 1) * NQ] = res.results[i]["out_s"]
    return yp, ys


def kernel(**inputs):
    return run(inputs, 4096)
```
